# Optimizing a Trainium2 kernel written in Bass

```python
import jax, jax.numpy as jnp
from jax import lax
import numpy as np

D_MODEL = 4096
BATCH = 4
SEQ = 2048
DEPTH = 1
DEC_BATCH = 128
DEC_SEQ = 4
PAST_LEN = 2048
PAGE_SIZE = 128

HEAD_DIM = 128
N_MIX_HEADS = D_MODEL // HEAD_DIM
N_CONV_GROUPS = N_MIX_HEADS // 2
CONV_DIM = N_CONV_GROUPS * HEAD_DIM
N_Q_HEADS = N_MIX_HEADS - N_CONV_GROUPS
N_KV_HEADS = max(1, N_Q_HEADS // 4)
GQA = N_Q_HEADS // N_KV_HEADS
ATTN_DIM = N_Q_HEADS * HEAD_DIM
KV_DIM = N_KV_HEADS * HEAD_DIM
W_IN_COLS = 3 * CONV_DIM + ATTN_DIM + 6 * KV_DIM + 3 * N_Q_HEADS
CONV_WIDTH = 3
L_CMP = 32
CMP_STRIDE = 16
R_CMP = L_CMP // CMP_STRIDE
D_PHI = HEAD_DIM
L_SEL = 64
N_SEL = 8
WINDOW = 512
Q_BLK = 128
SEL_Q_BLK = 32
FORCE_BONUS = 1e3
SCALE = HEAD_DIM ** -0.5
N_GROUPS = 4
EXPERTS_PER_GROUP = 4
N_EXPERTS = N_GROUPS * EXPERTS_PER_GROUP
TOP_K = 2
D_FF_EXPERT = D_MODEL // 8
RMS_EPS = 1e-6
NEG_INF = -1e30
TINY = 1e-30

kernel_name = 'hymba_conv_nsa_hmoe_step'


def rmsnorm(x, g):
    xf = x.astype(jnp.float32)
    inv = lax.rsqrt(jnp.mean(xf * xf, axis=-1, keepdims=True) + RMS_EPS)
    return (xf * inv).astype(x.dtype) * g


def masked_softmax(s, mask):
    s = jnp.where(mask, s.astype(jnp.float32), NEG_INF)
    m = jnp.max(s, axis=-1, keepdims=True)
    e = jnp.where(mask, jnp.exp(s - m), 0.0)
    return e / jnp.maximum(jnp.sum(e, axis=-1, keepdims=True), TINY)


def qk_norm_keys(kv, g):
    return jnp.stack([rmsnorm(kv[:, :, 0], g), kv[:, :, 1]], axis=2)


def mix_project(x, lw):
    n, t = x.shape[:2]
    z = rmsnorm(x, lw['norm_mix_g']) @ lw['w_in']
    sizes = (CONV_DIM, CONV_DIM, CONV_DIM, ATTN_DIM, 2 * KV_DIM, 2 * KV_DIM, 2 * KV_DIM, 3 * N_Q_HEADS)
    cuts = [int(c) for c in np.cumsum(sizes)[:-1]]
    gb, gc, hc, q, kvc, kvs, kvw, g = jnp.split(z, cuts, axis=-1)
    q = rmsnorm(q.reshape(n, t, N_Q_HEADS, HEAD_DIM), lw['q_norm_g'])
    kv_shape = (n, t, 2, N_KV_HEADS, HEAD_DIM)
    kvc = kvc.reshape(kv_shape)
    kvs = qk_norm_keys(kvs.reshape(kv_shape), lw['k_norm_g'][1])
    kvw = qk_norm_keys(kvw.reshape(kv_shape), lw['k_norm_g'][2])
    gates = jax.nn.sigmoid(g.reshape(n, t, 3, N_Q_HEADS))
    return gb, gc, hc, q, kvc, kvs, kvw, gates


def short_conv(b, c, h, prefix, conv_w):
    u = c * h
    t = u.shape[1]
    up = jnp.concatenate([prefix.astype(u.dtype), u], axis=1)
    y = up[:, 0:t] * conv_w[0]
    for j in range(1, CONV_WIDTH):
        y = y + up[:, j:j + t] * conv_w[j]
    return b * y, up[:, t:]


def chunk_contrib(rows, w1):
    n, t = rows.shape[:2]
    ch = rows.reshape(n, t // CMP_STRIDE, CMP_STRIDE, 2, N_KV_HEADS, HEAD_DIM)
    w1r = w1.reshape(2, R_CMP, CMP_STRIDE, HEAD_DIM, D_PHI)
    return jnp.einsum('ncsvjd,vrsde->ncrvje', ch, w1r)


def compress_blocks(contrib, lw):
    nb = contrib.shape[1] - R_CMP + 1
    hpre = contrib[:, 0:nb, 0]
    for r in range(1, R_CMP):
        hpre = hpre + contrib[:, r:r + nb, r]
    pe_bias = jnp.einsum('vld,vlde->ve', lw['phi_pe'], lw['phi_w1'])
    hid = jax.nn.gelu(hpre + pe_bias[:, None, :])
    out = jnp.einsum('nbvje,ved->nbvjd', hid, lw['phi_w2'])
    return rmsnorm(out[:, :, 0], lw['k_norm_g'][0]), out[:, :, 1]


def cmp_attention(q, pos, k_c, v_c):
    n, t = q.shape[:2]
    nb = k_c.shape[1]
    qg = q.reshape(n, t, N_KV_HEADS, GQA, HEAD_DIM)
    s = jnp.einsum('nqkgd,nbkd->nkgqb', qg, k_c) * SCALE
    mask = (jnp.arange(nb) * CMP_STRIDE + L_CMP - 1)[None, :] <= pos[:, None]
    p = masked_softmax(s, mask)
    o = jnp.einsum('nkgqb,nbkd->nqkgd', p.astype(v_c.dtype), v_c)
    return o.reshape(n, t, N_Q_HEADS, HEAD_DIM), p


def block_cover_matrix(nb, nsb):
    i = np.arange(nb)[:, None]
    j = np.arange(nsb)[None, :]
    m = np.zeros((nb, nsb), np.float32)
    for a in range(L_SEL // CMP_STRIDE):
        for c in range(R_CMP):
            m += (i == (L_SEL // CMP_STRIDE) * j + a - c)
    return jnp.asarray(m)


def select_blocks(p_cmp, pos, nsb):
    imp = jnp.einsum('nkgqb,bj->nkqj', p_cmp, block_cover_matrix(p_cmp.shape[-1], nsb))
    blk = jnp.arange(nsb)[None, :]
    cur = (pos // L_SEL)[:, None]
    forced = (blk == 0) | (blk == cur) | (blk == cur - 1)
    score = jnp.where(blk <= cur, imp + jnp.where(forced, FORCE_BONUS, 0.0), NEG_INF)
    return lax.top_k(score, min(N_SEL, nsb))[1]


def sel_attention(q, pos, idx, k_g, v_g):
    n, t = q.shape[:2]
    ns = idx.shape[-1]
    qg = q.reshape(n, t, N_KV_HEADS, GQA, HEAD_DIM)
    s = jnp.einsum('nqkgd,nkqsld->nkgqsl', qg, k_g) * SCALE
    kpos = idx[..., None] * L_SEL + jnp.arange(L_SEL)
    mask = (kpos <= pos[:, None, None]).reshape(n, N_KV_HEADS, 1, t, ns * L_SEL)
    p = masked_softmax(s.reshape(n, N_KV_HEADS, GQA, t, ns * L_SEL), mask)
    o = jnp.einsum('nkgqsl,nkqsld->nqkgd', p.reshape(s.shape).astype(v_g.dtype), v_g)
    return o.reshape(n, t, N_Q_HEADS, HEAD_DIM)


def window_attention_prompt(q, kv_win):
    n, t = q.shape[:2]
    nqb, nw = t // Q_BLK, WINDOW // Q_BLK
    padded = jnp.pad(kv_win, ((0, 0), (WINDOW, 0), (0, 0), (0, 0), (0, 0)))
    blocks = padded.reshape(n, nqb + nw, Q_BLK, 2, N_KV_HEADS, HEAD_DIM)
    band = jnp.concatenate([blocks[:, j:j + nqb] for j in range(nw + 1)], axis=2)
    qg = q.reshape(n, nqb, Q_BLK, N_KV_HEADS, GQA, HEAD_DIM)
    s = jnp.einsum('biqkgd,bimkd->bkgiqm', qg, band[:, :, :, 0]) * SCALE
    qpos = jnp.arange(t).reshape(nqb, Q_BLK)[:, :, None]
    kpos = ((jnp.arange(nqb) - nw) * Q_BLK)[:, None, None] + jnp.arange((nw + 1) * Q_BLK)[None, None, :]
    mask = (kpos >= 0) & (kpos <= qpos) & (kpos > qpos - WINDOW)
    p = masked_softmax(s, mask)
    o = jnp.einsum('bkgiqm,bimkd->biqkgd', p.astype(band.dtype), band[:, :, :, 1])
    return o.reshape(n, t, N_Q_HEADS, HEAD_DIM)


def combine_branches(gates, o_cmp, o_sel, o_win):
    return (gates[:, :, 0, :, None] * o_cmp + gates[:, :, 1, :, None] * o_sel
            + gates[:, :, 2, :, None] * o_win)


def nsa_prompt(q, kvc, kvs, kvw, gates, lw):
    n, t = q.shape[:2]
    pos = jnp.arange(t)
    k_c, v_c = compress_blocks(chunk_contrib(kvc, lw['phi_w1']), lw)
    o_cmp, p_cmp = cmp_attention(q, pos, k_c, v_c)
    nsb = t // L_SEL
    idx = select_blocks(p_cmp, pos, nsb)
    ns = idx.shape[-1]
    kvb = kvs.reshape(n, nsb, L_SEL, 2, N_KV_HEADS, HEAD_DIM)
    b_idx = jnp.arange(n)[:, None, None, None]
    h_idx = jnp.arange(N_KV_HEADS)[None, :, None, None]

    def sel_block(args):
        qb, ib, pb = args
        g = kvb[b_idx, ib, :, :, h_idx]
        return sel_attention(qb, pb, ib, g[..., 0, :], g[..., 1, :])

    nqb = t // SEL_Q_BLK
    qbs = q.reshape(n, nqb, SEL_Q_BLK, N_Q_HEADS, HEAD_DIM).transpose(1, 0, 2, 3, 4)
    ibs = idx.reshape(n, N_KV_HEADS, nqb, SEL_Q_BLK, ns).transpose(2, 0, 1, 3, 4)
    pbs = pos.reshape(nqb, SEL_Q_BLK)
    o_sel = lax.map(sel_block, (qbs, ibs, pbs)).transpose(1, 0, 2, 3, 4).reshape(n, t, N_Q_HEADS, HEAD_DIM)
    o_win = window_attention_prompt(q, kvw)
    return combine_branches(gates, o_cmp, o_sel, o_win)


def nsa_sample(q, kvc, kvs, kvw, gates, cache_cmp, cache_sel, win_buf, page_table, lw):
    n, t = q.shape[:2]
    pos = PAST_LEN + jnp.arange(t)
    qg = q.reshape(n, t, N_KV_HEADS, GQA, HEAD_DIM)
    past_cmp = cache_cmp[page_table].reshape(n, -1, 2, N_KV_HEADS, HEAD_DIM)
    contrib = chunk_contrib(past_cmp, lw['phi_w1'])
    n_new_ch = t // CMP_STRIDE
    if n_new_ch > 0:
        contrib = jnp.concatenate([contrib, chunk_contrib(kvc[:, :n_new_ch * CMP_STRIDE], lw['phi_w1'])], axis=1)
    k_c, v_c = compress_blocks(contrib, lw)
    o_cmp, p_cmp = cmp_attention(q, pos, k_c, v_c)
    npb = PAST_LEN // L_SEL
    n_new_blk = -(-t // L_SEL)
    idx = select_blocks(p_cmp, pos, npb + n_new_blk)
    ns = idx.shape[-1]
    bpp = PAGE_SIZE // L_SEL
    pool_blk = cache_sel.reshape(cache_sel.shape[0], bpp, L_SEL, 2, N_KV_HEADS, HEAD_DIM)
    n_idx = jnp.arange(n)[:, None, None, None]
    h_idx = jnp.arange(N_KV_HEADS)[None, :, None, None]
    pidx = jnp.minimum(idx, npb - 1)
    phys = page_table[n_idx, pidx // bpp]
    g = pool_blk[phys, pidx % bpp, :, :, h_idx]
    s_past = (jnp.einsum('nqkgd,nkqsld->nkgqsl', qg, g[..., 0, :]) * SCALE).reshape(n, N_KV_HEADS, GQA, t, ns * L_SEL)
    kpos_p = idx[..., None] * L_SEL + jnp.arange(L_SEL)
    mask_past = ((idx < npb)[..., None] & (kpos_p <= pos[:, None, None])).reshape(n, N_KV_HEADS, t, ns * L_SEL)
    m_new = n_new_blk * L_SEL
    new_rows = jnp.pad(kvs, ((0, 0), (0, m_new - t), (0, 0), (0, 0), (0, 0)))
    new_blk_id = npb + jnp.arange(m_new) // L_SEL
    picked = jnp.any(idx[..., None] == new_blk_id, axis=-2)
    mask_new = picked & ((PAST_LEN + jnp.arange(m_new))[None, :] <= pos[:, None])
    s_new = jnp.einsum('nqkgd,nmkd->nkgqm', qg, new_rows[:, :, 0]) * SCALE
    p = masked_softmax(jnp.concatenate([s_past, s_new], axis=-1),
                       jnp.concatenate([mask_past, mask_new], axis=-1)[:, :, None])
    p_past = p[..., :ns * L_SEL].reshape(n, N_KV_HEADS, GQA, t, ns, L_SEL).astype(g.dtype)
    o_sel = (jnp.einsum('nkgqsl,nkqsld->nqkgd', p_past, g[..., 1, :])
             + jnp.einsum('nkgqm,nmkd->nqkgd', p[..., ns * L_SEL:].astype(new_rows.dtype), new_rows[:, :, 1]))
    o_sel = o_sel.reshape(n, t, N_Q_HEADS, HEAD_DIM)
    keys = jnp.concatenate([win_buf.astype(kvw.dtype), kvw], axis=1)
    w_buf = win_buf.shape[1]
    kpos = PAST_LEN - w_buf + jnp.arange(w_buf + t)
    mask = (kpos[None, :] <= pos[:, None]) & (kpos[None, :] > pos[:, None] - WINDOW)
    s = jnp.einsum('nqkgd,nmkd->nkgqm', qg, keys[:, :, 0]) * SCALE
    pw = masked_softmax(s, mask)
    o_win = jnp.einsum('nkgqm,nmkd->nqkgd', pw.astype(keys.dtype), keys[:, :, 1]).reshape(n, t, N_Q_HEADS, HEAD_DIM)
    return combine_branches(gates, o_cmp, o_sel, o_win), keys[:, -w_buf:]


def mix_output(x, conv_out, attn_out, lw):
    n, t = x.shape[:2]
    g = lw['out_norm_g']
    merged = jnp.concatenate([rmsnorm(conv_out, g[:CONV_DIM]),
                              rmsnorm(attn_out.reshape(n, t, ATTN_DIM), g[CONV_DIM:])], axis=-1)
    return x + merged @ lw['w_out']


def moe_ffn(h, lw):
    n, t, d = h.shape
    xt = rmsnorm(h, lw['norm_ffn_g']).reshape(n * t, d)
    p_grp = jax.nn.softmax((xt @ lw['w_group_router']).astype(jnp.float32) + lw['b_group_router'], axis=-1)
    g_val, g_idx = lax.top_k(p_grp, 1)
    e_logit = ((xt @ lw['w_expert_router']).astype(jnp.float32)
               + lw['b_expert_router']).reshape(n * t, N_GROUPS, EXPERTS_PER_GROUP)
    e_logit = jnp.take_along_axis(e_logit, g_idx[:, :, None], axis=1)[:, 0]
    e_val, e_idx = lax.top_k(jax.nn.softmax(e_logit, axis=-1), TOP_K)
    w = g_val * e_val / jnp.sum(e_val, axis=-1, keepdims=True)
    gate = jnp.einsum('tk,tke->te', w,
                      jax.nn.one_hot(g_idx * EXPERTS_PER_GROUP + e_idx, N_EXPERTS, dtype=jnp.float32))
    hid = jax.nn.silu(jnp.einsum('td,edf->tef', xt, lw['w_gate'])) * jnp.einsum('td,edf->tef', xt, lw['w_up'])
    out = jnp.einsum('tef,efd->td', hid * gate[:, :, None].astype(hid.dtype), lw['w_down'])
    return h + out.reshape(n, t, d)


def layer_prompt(x, lw):
    n, t = x.shape[:2]
    gb, gc, hc, q, kvc, kvs, kvw, gates = mix_project(x, lw)
    conv_out, conv_state = short_conv(gb, gc, hc, jnp.zeros((n, CONV_WIDTH - 1, CONV_DIM), x.dtype), lw['conv_w'])
    attn_out = nsa_prompt(q, kvc, kvs, kvw, gates, lw)
    y = moe_ffn(mix_output(x, conv_out, attn_out, lw), lw)
    return y, kvc, kvs, kvw[:, t - min(WINDOW, t):], conv_state


def layer_sample(x, cache_cmp, cache_sel, win_buf, conv_buf, page_table, lw):
    gb, gc, hc, q, kvc, kvs, kvw, gates = mix_project(x, lw)
    conv_out, conv_state = short_conv(gb, gc, hc, conv_buf, lw['conv_w'])
    attn_out, new_win = nsa_sample(q, kvc, kvs, kvw, gates, cache_cmp, cache_sel, win_buf, page_table, lw)
    y = moe_ffn(mix_output(x, conv_out, attn_out, lw), lw)
    return y, kvc, kvs, new_win, conv_state


def setup_inputs(seed: int = 0) -> dict:
    key = jax.random.key(seed)
    ks = jax.random.split(key, 26)
    f32 = jnp.float32

    def nrm(k, shape, scale=1.0):
        return jax.random.normal(k, shape, f32) * scale

    def gain(k, shape):
        return 1.0 + 0.01 * jax.random.normal(k, shape, f32)

    n_pages = PAST_LEN // PAGE_SIZE
    n_used = DEC_BATCH * n_pages
    n_pool = n_used + n_used // 4
    w_buf = min(WINDOW, PAST_LEN)
    page_table = jax.random.permutation(ks[6], n_pool)[:n_used].reshape(DEC_BATCH, n_pages).astype(jnp.int32)
    pool_shape = (DEPTH, n_pool, PAGE_SIZE, 2, N_KV_HEADS, HEAD_DIM)
    return {
        'x_prompt': nrm(ks[0], (BATCH, SEQ, D_MODEL)),
        'x_sample': nrm(ks[1], (DEC_BATCH, DEC_SEQ, D_MODEL)),
        'cache_cmp_kv': nrm(ks[2], pool_shape),
        'cache_sel_kv': nrm(ks[3], pool_shape),
        'state_win_kv': nrm(ks[4], (DEPTH, DEC_BATCH, w_buf, 2, N_KV_HEADS, HEAD_DIM)),
        'state_conv': nrm(ks[5], (DEPTH, DEC_BATCH, CONV_WIDTH - 1, CONV_DIM)),
        'page_table': page_table,
        'norm_mix_g': gain(ks[7], (DEPTH, D_MODEL)),
        'w_in': nrm(ks[8], (DEPTH, D_MODEL, W_IN_COLS), D_MODEL ** -0.5),
        'conv_w': nrm(ks[9], (DEPTH, CONV_WIDTH, CONV_DIM), CONV_WIDTH ** -0.5),
        'q_norm_g': gain(ks[10], (DEPTH, HEAD_DIM)),
        'k_norm_g': gain(ks[11], (DEPTH, 3, HEAD_DIM)),
        'phi_pe': nrm(ks[12], (DEPTH, 2, L_CMP, HEAD_DIM), 0.1),
        'phi_w1': nrm(ks[13], (DEPTH, 2, L_CMP, HEAD_DIM, D_PHI), (L_CMP * HEAD_DIM) ** -0.5),
        'phi_w2': nrm(ks[14], (DEPTH, 2, D_PHI, HEAD_DIM), D_PHI ** -0.5),
        'out_norm_g': gain(ks[15], (DEPTH, D_MODEL)),
        'w_out': nrm(ks[16], (DEPTH, D_MODEL, D_MODEL), D_MODEL ** -0.5),
        'norm_ffn_g': gain(ks[17], (DEPTH, D_MODEL)),
        'w_group_router': nrm(ks[18], (DEPTH, D_MODEL, N_GROUPS), D_MODEL ** -0.5),
        'b_group_router': nrm(ks[19], (DEPTH, N_GROUPS), 0.01),
        'w_expert_router': nrm(ks[20], (DEPTH, D_MODEL, N_EXPERTS), D_MODEL ** -0.5),
        'b_expert_router': nrm(ks[21], (DEPTH, N_EXPERTS), 0.01),
        'w_gate': nrm(ks[22], (DEPTH, N_EXPERTS, D_MODEL, D_FF_EXPERT), D_MODEL ** -0.5),
        'w_up': nrm(ks[23], (DEPTH, N_EXPERTS, D_MODEL, D_FF_EXPERT), D_MODEL ** -0.5),
        'w_down': nrm(ks[24], (DEPTH, N_EXPERTS, D_FF_EXPERT, D_MODEL), D_FF_EXPERT ** -0.5),
    }


def reference(x_prompt, x_sample, cache_cmp_kv, cache_sel_kv, state_win_kv, state_conv, page_table,
              norm_mix_g, w_in, conv_w, q_norm_g, k_norm_g, phi_pe, phi_w1, phi_w2, out_norm_g, w_out,
              norm_ffn_g, w_group_router, b_group_router, w_expert_router, b_expert_router,
              w_gate, w_up, w_down):
    y_prompt, y_sample = x_prompt, x_sample
    p_cmp, p_sel, p_win, p_conv = [], [], [], []
    s_cmp, s_sel, s_win, s_conv = [], [], [], []
    for l in range(DEPTH):
        lw = dict(norm_mix_g=norm_mix_g[l], w_in=w_in[l], conv_w=conv_w[l], q_norm_g=q_norm_g[l],
                  k_norm_g=k_norm_g[l], phi_pe=phi_pe[l], phi_w1=phi_w1[l], phi_w2=phi_w2[l],
                  out_norm_g=out_norm_g[l], w_out=w_out[l], norm_ffn_g=norm_ffn_g[l],
                  w_group_router=w_group_router[l], b_group_router=b_group_router[l],
                  w_expert_router=w_expert_router[l], b_expert_router=b_expert_router[l],
                  w_gate=w_gate[l], w_up=w_up[l], w_down=w_down[l])
        y_prompt, a, b, c, d = layer_prompt(y_prompt, lw)
        p_cmp.append(a); p_sel.append(b); p_win.append(c); p_conv.append(d)
        y_sample, a, b, c, d = layer_sample(y_sample, cache_cmp_kv[l], cache_sel_kv[l], state_win_kv[l],
                                            state_conv[l], page_table, lw)
        s_cmp.append(a); s_sel.append(b); s_win.append(c); s_conv.append(d)
    return (y_prompt, y_sample, jnp.stack(p_cmp), jnp.stack(p_sel), jnp.stack(p_win), jnp.stack(p_conv),
            jnp.stack(s_cmp), jnp.stack(s_sel), jnp.stack(s_win), jnp.stack(s_conv))
```

```python
import numpy as np
from contextlib import ExitStack
import concourse.bass as bass
import concourse.mybir as mybir
from concourse.bass_utils import run_bass_kernel_spmd

F32 = mybir.dt.float32
BF16 = mybir.dt.bfloat16
I32 = mybir.dt.int32
AF = mybir.ActivationFunctionType
ALU = mybir.AluOpType
AX = mybir.AxisListType

NCORES = 8
D = 4096
NKC = 32
HD = 128
SEQ = 2048
HALF = 1024
NS = 16
NST = 64
NA = HALF + NST + 2
NROW = SEQ + NST + 2
NOUT = HALF + NST
WIN_COLS = 11312
C_GB, C_GC, C_HC, C_Q, C_KV, C_G = 0, 2048, 4096, 6144, 8192, 11264
SCALE = HD ** -0.5
EPS = 1e-6
NEG = -1e30
CAP_C = 30000
CAP_D = 1900
NSEM_C = 4
NSEM_D = 8


class Prog:
    ENG = ('sp', 'act', 'pool', 'dve', 'pe')

    def __init__(self, nc, st):
        self.nc = nc
        self.cnt = {}
        self.emitted = {}
        self.sems = {}
        for e in self.ENG:
            if e != 'sp':
                self.sems[(e, 'c')] = [st.enter_context(nc.semaphore(f"s_{e}_c{i}")) for i in range(NSEM_C)]
            if e in ('sp', 'pool', 'act'):
                self.sems[(e, 'd')] = [st.enter_context(nc.semaphore(f"s_{e}_d{i}")) for i in range(NSEM_D)]
        self.reset()

    def reset(self):
        self.ops = {e: [] for e in self.ENG}
        self.lastw = {}
        self.readers = {}
        self.wm = {}

    def add(self, eng, fn, r=(), w=(), dma=False):
        kind = 'd' if dma else 'c'
        w = list(w) + [k for k in r if k.startswith('ps') and k[2:].isdigit()]
        deps = set()
        for k in r:
            if k in self.lastw:
                deps.add(self.lastw[k])
        for k in w:
            if k in self.lastw:
                deps.add(self.lastw[k])
            for x in self.readers.get(k, ()):
                deps.add(x)
        need = {}
        for (se, sk, si) in deps:
            if se == eng and eng == 'pe' and sk == 'c' and kind == 'c':
                continue
            need[(se, sk)] = max(need.get((se, sk), -1), si)
        waits = []
        for (se, sk), si in need.items():
            if self.wm.get((eng, se, sk), -1) >= si:
                continue
            self.wm[(eng, se, sk)] = si
            waits.append((se, sk, si))
        idx = self.cnt.get((eng, kind), 0)
        self.cnt[(eng, kind)] = idx + 1
        me = (eng, kind, idx)
        for k in r:
            self.readers.setdefault(k, []).append(me)
        for k in w:
            self.lastw[k] = me
            self.readers[k] = []
        self.ops[eng].append((waits, fn, kind, idx))

    def _semval(self, kind, idx):
        cap = CAP_D if kind == 'd' else CAP_C
        return idx // cap, ((idx % cap) + 1) * (16 if kind == 'd' else 1)

    def emit(self):
        nc = self.nc
        with nc.Block() as block:
            def run(name, e):
                for (waits, fn, kind, idx) in self.ops[name]:
                    for (se, sk, si) in waits:
                        ep, v = self._semval(sk, si)
                        e.wait_ge(self.sems[(se, sk)][ep], v)
                    ep, v = self._semval(kind, idx)
                    if kind == 'd' and idx > 0 and idx % CAP_D == 0:
                        e.wait_ge(self.sems[(name, kind)][ep - 1], CAP_D * 16)
                    ins = fn(e)
                    ins.then_inc(self.sems[(name, kind)][ep], 16 if kind == 'd' else 1)
                n = self.cnt.get((name, 'd'), 0)
                if n:
                    ep, v = self._semval('d', n - 1)
                    e.wait_ge(self.sems[(name, 'd')][ep], v)

            @block.sync
            def _(e):
                run('sp', e)

            @block.scalar
            def _(e):
                run('act', e)

            @block.gpsimd
            def _(e):
                run('pool', e)

            @block.vector
            def _(e):
                run('dve', e)

            @block.tensor
            def _(e):
                run('pe', e)
        nc.all_engine_barrier()
        self.reset()


def bc(ap, shape):
    return ap.to_broadcast(shape)


import os
DBG_BR = int(os.environ.get('DBG_BR', '-1'))
DBG_HEAD = int(os.environ.get('DBG_HEAD', '5'))
DBG_DUMP = int(os.environ.get('DBG_DUMP', '0'))


def build(upto=99):
    nc = bass.Bass("TRN2", target_bir_lowering=False)
    dt_in = lambda n, s, d=F32: nc.dram_tensor(n, s, d, kind="ExternalInput").ap()
    dt_out = lambda n, s, d=F32: nc.dram_tensor(n, s, d, kind="ExternalOutput").ap()
    xs = dt_in("xs", [NROW, D])
    w_in = dt_in("w_in", [D, WIN_COLS])
    norm_mix_g = dt_in("norm_mix_g", [32, 128])
    out_norm_g = dt_in("out_norm_g", [32, 128])
    norm_ffn_g = dt_in("norm_ffn_g", [32, 128])
    k_norm_g = dt_in("k_norm_g", [3, 128])
    q_norm_g = dt_in("q_norm_g", [1, 128])
    conv_w = dt_in("conv_w", [48, 128])
    phi_pe = dt_in("phi_pe", [64, 128])
    phi_w1 = dt_in("phi_w1", [2, 32, 128, 128])
    phi_w2 = dt_in("phi_w2", [2, 128, 128])
    state_conv = dt_in("state_conv", [512, 128])
    b_r = dt_in("b_r", [1, 20])
    if upto >= 5:
        state_win = dt_in("state_win", [NS, 512, 1024])
        cache_cmp = dt_in("cache_cmp", [2560 * 128, 1024])
        cache_sel = dt_in("cache_sel", [2560 * 128, 1024])
        page_table = dt_in("page_table", [1, 256], I32)
    if upto >= 6:
        w_out = dt_in("w_out", [D, D])
    if upto >= 7:
        w_gr = dt_in("w_gr", [D, 4])
        w_er = dt_in("w_er", [D, 16])
        w_gate = dt_in("w_gate", [16, D, 512])
        w_up = dt_in("w_up", [16, D, 512])
        w_down = dt_in("w_down", [16, 512, D])
    m_cmpT = dt_in("m_cmpT", [128, HALF], BF16)
    m_MZ = dt_in("m_MZ", [128, 33], BF16)
    m_add = dt_in("m_add", [128, 8, 32])
    m_cblk = dt_in("m_cblk", [128, 8, 32])
    m_E = dt_in("m_E", [32, SEQ], BF16)
    m_tri = dt_in("m_tri", [128, 256], BF16)
    m_flag = dt_in("m_flag", [128, 1])
    s_MZ = dt_in("s_MZ", [128, 34], BF16)
    s_add = dt_in("s_add", [64, 40])
    s_misc = dt_in("s_misc", [128, 64])
    s_selN = dt_in("s_selN", [64, NS * 128], BF16)

    y_o = dt_out("y", [NOUT, D])
    kv_o = [dt_out(f"kv{b}", [SEQ + NST, 1024]) for b in range(3)]
    win_o = dt_out("win_o", [NS, 512, 1024])
    conv_o = dt_out("conv_o", [16, 128, 34])
    dbg = dt_out("dbg", [128, 4096]) if DBG_DUMP else None

    convT_s = nc.dram_tensor("convT_s", [16, 128, NOUT], BF16).ap()
    h_s = nc.dram_tensor("h_s", [NOUT, D], F32).ap()

    uid = [0]

    def sb(st, name, shape, dt=F32):
        uid[0] += 1
        t = st.enter_context(nc.sbuf_tensor(f"{name}_{uid[0]}", shape, dt))
        nb = int(np.prod(shape[1:])) * (4 if dt in (F32, I32) else 2)
        if nb % 64:
            st.enter_context(nc.sbuf_tensor(f"pad_{uid[0]}", [128, (64 - nb % 64) // 2], BF16))
        return t

    with ExitStack() as st0:
        P = Prog(nc, st0)
        A = P.add
        ps = [st0.enter_context(nc.psum_tensor(f"ps{i}", [128, 512], F32)) for i in range(8)]
        psb = [p[:].bitcast(BF16) for p in ps]
        ident = sb(st0, "ident", [128, 128], BF16)
        identf = sb(st0, "identf", [128, 128])
        ones_b = sb(st0, "ones_b", [128, 128], BF16)
        ones_f = sb(st0, "ones_f", [128, 128])
        epsc = sb(st0, "epsc", [128, 1])
        tinyc = sb(st0, "tinyc", [128, 1])
        cA = sb(st0, "cA", [128, 128])
        cB = sb(st0, "cB", [128, 128])
        scT = sb(st0, "scT", [128, 512])
        gk_bc = sb(st0, "gk_bc", [128, 3, 128])
        br_bc = sb(st0, "br_bc", [128, 20])
        inv_c = sb(st0, "inv_c", [128, 9])
        flag = sb(st0, "flag", [128, 1])
        inv_a = sb(st0, "inv_a", [128, 9])
        st_mid = ExitStack()
        qT = sb(st_mid, "qT", [128, 16, NA], BF16)
        gates = sb(st_mid, "gates", [128, 9, 48])

        with ExitStack() as st:
            stA = sb(st, "stA", [128, 128])
            stB = sb(st, "stB", [128, 128])
            stC = sb(st, "stC", [128, 4, 128])
            A('pool', lambda e: e.memset(identf[:], 0.0), w=['identf'])
            A('pool', lambda e: e.affine_select(out=identf[:], in_=identf[:], pattern=[[-1, 128]], compare_op=ALU.not_equal,
                                                fill=1.0, base=0, channel_multiplier=1), r=['identf'], w=['identf'])
            A('dve', lambda e: e.tensor_copy(ident[:], identf[:]), r=['identf'], w=['ident'])
            A('dve', lambda e: e.memset(ones_b[:], 1.0), w=['ones_b'])
            A('dve', lambda e: e.memset(ones_f[:], 1.0), w=['ones_f'])
            A('dve', lambda e: e.memset(epsc[:], EPS), w=['epsc'])
            A('dve', lambda e: e.memset(tinyc[:], 1e-30), w=['tinyc'])
            A('dve', lambda e: e.memset(stA[:], 0.0), w=['stA'])
            A('dve', lambda e: e.memset(stB[:], 0.0), w=['stB'])
            for (src, r0, n) in ((norm_mix_g, 0, 32), (out_norm_g, 32, 32), (norm_ffn_g, 64, 32), (k_norm_g, 96, 3), (q_norm_g, 99, 1)):
                A('sp', lambda e, src=src, r0=r0, n=n: e.dma_start(out=stA[r0:r0 + n, :], in_=src), w=['stA'], dma=True)
            A('sp', lambda e: e.dma_start(out=stB[0:48, :], in_=conv_w), w=['stB'], dma=True)
            A('sp', lambda e: e.dma_start(out=stB[64:128, :], in_=phi_pe), w=['stB'], dma=True)
            A('sp', lambda e: e.dma_start(out=stC[:], in_=state_conv.rearrange("(t p) d -> p t d", p=128)), w=['stC'], dma=True)
            A('sp', lambda e: e.dma_start(out=gk_bc[:].rearrange("p a b -> p (a b)"), in_=k_norm_g.rearrange("a b -> (a b)").unsqueeze(0).partition_broadcast(128)), w=['gk_bc'], dma=True)
            A('sp', lambda e: e.dma_start(out=br_bc[:], in_=b_r.partition_broadcast(128)), w=['br_bc'], dma=True)
            A('sp', lambda e: e.dma_start(out=flag[:], in_=m_flag), w=['flag'], dma=True)
            A('pe', lambda e: e.transpose(ps[0][:, 0:128], stA[:], identf[:]), r=['stA', 'identf'], w=['ps0'])
            A('dve', lambda e: e.tensor_copy(cA[:], ps[0][:, 0:128]), r=['ps0'], w=['cA'])
            A('pe', lambda e: e.transpose(ps[1][:, 0:128], stB[:], identf[:]), r=['stB', 'identf'], w=['ps1'])
            A('dve', lambda e: e.tensor_copy(cB[:], ps[1][:, 0:128]), r=['ps1'], w=['cB'])
            for t in range(4):
                A('pe', lambda e, t=t: e.transpose(ps[2][:, t * 128:(t + 1) * 128], stC[:, t, :], identf[:]), r=['stC', 'identf'], w=['ps2'])
            A('dve', lambda e: e.tensor_copy(scT[:], ps[2][:]), r=['ps2'], w=['scT'])
            P.emit()

        gmixT, goutT, gffnT = cA[:, 0:32], cA[:, 32:64], cA[:, 64:96]
        TILES_A = [(i, 128 * i, 128, 128 * i) for i in range(8)] + [(16, SEQ, 66, HALF)]
        TILES_B = [(i, 128 * i, 128, 128 * (i - 8)) for i in range(8, 16)]

        with ExitStack() as stx:
            xnT = sb(stx, "xnT", [128, NKC, NA], BF16)

            def xnorm(tiles):
                with ExitStack() as st:
                    xin = [sb(st, f"xin{i}", [128, D]) for i in range(2)]
                    xsb = [sb(st, f"xsb{i}", [128, D], BF16) for i in range(2)]
                    ssq = sb(st, "ssq", [128, 32])
                    for n, (ti, r0, R, c0) in enumerate(tiles):
                        s = n % 2
                        A('sp', lambda e, s=s, r0=r0, R=R: e.dma_start(out=xin[s][0:R, :], in_=xs[r0:r0 + R, :]), w=[f'xin{s}'], dma=True)
                        A('dve', lambda e, n=n: e.memset(ssq[:, n:n + 1], 0.0), w=[f'ssq{n}'])
                        A('act', lambda e, s=s, R=R, n=n: e.activation(out=xsb[s][0:R, :], in_=xin[s][0:R, :], func=AF.Square, accum_out=ssq[0:R, n:n + 1]),
                          r=[f'xin{s}'], w=[f'xsb{s}', f'ssq{n}'])
                        A('act', lambda e, R=R, n=n: e.activation(out=ssq[0:R, n:n + 1], in_=ssq[0:R, n:n + 1], func=AF.Sqrt, bias=epsc[0:R, :], scale=1.0 / D),
                          r=[f'ssq{n}', 'epsc'], w=[f'ssq{n}'])
                        A('dve', lambda e, R=R, n=n: e.reciprocal(ssq[0:R, n:n + 1], ssq[0:R, n:n + 1]), r=[f'ssq{n}'], w=[f'ssq{n}'])
                        A('dve', lambda e, s=s, R=R, n=n: e.tensor_scalar(xsb[s][0:R, :], xin[s][0:R, :], ssq[0:R, n:n + 1], None, op0=ALU.mult),
                          r=[f'xin{s}', f'ssq{n}'], w=[f'xsb{s}'])
                        for g in range(4):
                            b = (n * 4 + g) % 4
                            for c in range(8):
                                kc = g * 8 + c
                                A('pe', lambda e, s=s, R=R, b=b, c=c, kc=kc: e.transpose(psb[b][:, c * 128:c * 128 + R], xsb[s][0:R, kc * 128:(kc + 1) * 128], ident[0:R, 0:R]),
                                  r=[f'xsb{s}', 'ident'], w=[f'ps{b}'])
                            A('dve', lambda e, R=R, b=b, g=g, c0=c0: e.tensor_tensor(
                                out=xnT[:, g * 8:(g + 1) * 8, c0:c0 + R],
                                in0=psb[b].rearrange("p (c t) -> p c t", c=8)[:, :, 0:R],
                                in1=bc(gmixT[:, g * 8:(g + 1) * 8].unsqueeze(2), [128, 8, R]), op=ALU.mult),
                              r=[f'ps{b}', 'cA'], w=['xnT'])
                    P.emit()

            def kv_project(tiles, st):
                wt = [sb(st, f"wkv{i}", [128, NKC, 128], BF16) for i in range(4)]
                kvst = [sb(st, f"kvst{i}", [128, 128]) for i in range(4)]
                junk = sb(st, "kvjunk", [128, 128])
                kss = sb(st, "kss", [128, 512])
                n = 0
                for cg in range(24):
                    br, kv, hd = cg // 8, (cg % 8) // 4, cg % 4
                    s = cg % 4
                    col = C_KV + cg * 128
                    A('pool', lambda e, s=s, col=col: e.dma_start(out=wt[s][:], in_=w_in[:, col:col + 128].rearrange("(c p) n -> p c n", p=128)), w=[f'wkv{s}'], dma=True)
                    for (ti, r0, R, c0) in tiles:
                        b = n % 4
                        k4 = n % 4
                        kc_ = n % 512
                        n += 1
                        for c in range(NKC):
                            A('pe', lambda e, b=b, s=s, c=c, R=R, c0=c0: e.matmul(ps[b][0:R, 0:128], lhsT=xnT[:, c, c0:c0 + R], rhs=wt[s][:, c, :], start=(c == 0), stop=(c == NKC - 1)),
                              r=['xnT', f'wkv{s}'], w=[f'ps{b}'])
                        if kv == 0 and br >= 1:
                            A('dve', lambda e, kc_=kc_: e.memset(kss[:, kc_:kc_ + 1], 0.0), w=[f'kss{kc_}'])
                            A('act', lambda e, b=b, R=R, kc_=kc_: e.activation(out=junk[0:R, :], in_=ps[b][0:R, 0:128], func=AF.Square, accum_out=kss[0:R, kc_:kc_ + 1]),
                              r=[f'ps{b}'], w=['kvjunk', f'kss{kc_}'])
                            A('act', lambda e, R=R, kc_=kc_: e.activation(out=kss[0:R, kc_:kc_ + 1], in_=kss[0:R, kc_:kc_ + 1], func=AF.Sqrt, bias=epsc[0:R, :], scale=1.0 / HD),
                              r=[f'kss{kc_}', 'epsc'], w=[f'kss{kc_}'])
                            A('dve', lambda e, R=R, kc_=kc_: e.reciprocal(kss[0:R, kc_:kc_ + 1], kss[0:R, kc_:kc_ + 1]), r=[f'kss{kc_}'], w=[f'kss{kc_}'])
                            A('dve', lambda e, b=b, R=R, kc_=kc_, k4=k4, br=br: e.scalar_tensor_tensor(out=kvst[k4][0:R, :], in0=ps[b][0:R, 0:128], scalar=kss[0:R, kc_:kc_ + 1],
                                                                                                 in1=gk_bc[0:R, br, :], op0=ALU.mult, op1=ALU.mult),
                              r=[f'ps{b}', f'kss{kc_}', 'gk_bc'], w=[f'kvst{k4}'])
                        else:
                            A('act', lambda e, b=b, R=R, k4=k4: e.copy(kvst[k4][0:R, :], ps[b][0:R, 0:128]), r=[f'ps{b}'], w=[f'kvst{k4}'])
                        Ro = min(R, 128 if ti < 16 else NST)
                        oc = kv * 512 + hd * 128
                        A('sp', lambda e, br=br, r0=r0, Ro=Ro, oc=oc, k4=k4: e.dma_start(out=kv_o[br][r0:r0 + Ro, oc:oc + 128], in_=kvst[k4][0:Ro, :]),
                          r=[f'kvst{k4}'], w=[f'kvo{br}'], dma=True)

            xnorm(TILES_A)
            if upto >= 1:
                with ExitStack() as st:
                    kv_project(TILES_A, st)
                    TG = [(0, 512), (512, 512), (1024, NA - 1024)]
                    wt = [sb(st, f"wt{i}", [128, NKC, 128], BF16) for i in range(3)]
                    sq = [sb(st, f"sq{i}", [128, 512]) for i in range(2)]
                    rq = [sb(st, f"rq{i}", [128, 512]) for i in range(2)]
                    nwt = [0]

                    def load_w(col, ncols=128):
                        s = nwt[0] % 3
                        nwt[0] += 1
                        A('pool', lambda e, s=s, col=col, ncols=ncols: e.dma_start(out=wt[s][:, :, 0:ncols], in_=w_in[:, col:col + ncols].rearrange("(c p) n -> p c n", p=128)),
                          w=[f'wt{s}'], dma=True)
                        return s
                    n = 0
                    for hh in range(16):
                        s = load_w(C_Q + hh * 128)
                        for (t0, tn) in TG:
                            b = 4 + (n % 2) * 2
                            k2 = n % 2
                            n += 1
                            for c in range(NKC):
                                A('pe', lambda e, b=b, s=s, c=c, t0=t0, tn=tn: e.matmul(ps[b][:, 0:tn], lhsT=wt[s][:, c, :], rhs=xnT[:, c, t0:t0 + tn], start=(c == 0), stop=(c == NKC - 1)),
                                  r=['xnT', f'wt{s}'], w=[f'ps{b}'])
                            A('act', lambda e, b=b, k2=k2, tn=tn: e.activation(out=sq[k2][:, 0:tn], in_=ps[b][:, 0:tn], func=AF.Square), r=[f'ps{b}'], w=[f'sq{k2}'])
                            A('pe', lambda e, b=b, k2=k2, tn=tn: e.matmul(ps[b + 1][:, 0:tn], lhsT=ones_f[:], rhs=sq[k2][:, 0:tn], start=True, stop=True),
                              r=[f'sq{k2}', 'ones_f'], w=[f'ps{b + 1}'])
                            A('act', lambda e, b=b, k2=k2, tn=tn: e.activation(out=rq[k2][:, 0:tn], in_=ps[b + 1][:, 0:tn], func=AF.Sqrt, bias=epsc[:], scale=1.0 / HD),
                              r=[f'ps{b + 1}', 'epsc'], w=[f'rq{k2}'])
                            A('dve', lambda e, k2=k2, tn=tn: e.reciprocal(rq[k2][:, 0:tn], rq[k2][:, 0:tn]), r=[f'rq{k2}'], w=[f'rq{k2}'])
                            A('dve', lambda e, b=b, k2=k2, hh=hh, t0=t0, tn=tn: e.scalar_tensor_tensor(out=qT[:, hh, t0:t0 + tn], in0=ps[b][:, 0:tn], scalar=cA[:, 99:100], in1=rq[k2][:, 0:tn],
                                                                                                   op0=ALU.mult, op1=ALU.mult),
                              r=[f'ps{b}', f'rq{k2}', 'cA'], w=['qT'])
                    s = load_w(C_G, 48)
                    for n, (ti, r0, R, c0) in enumerate(TILES_A):
                        b = 4 + n % 4
                        for c in range(NKC):
                            A('pe', lambda e, b=b, s=s, c=c, R=R, c0=c0: e.matmul(ps[b][0:R, 0:48], lhsT=xnT[:, c, c0:c0 + R], rhs=wt[s][:, c, 0:48], start=(c == 0), stop=(c == NKC - 1)),
                              r=['xnT', f'wt{s}'], w=[f'ps{b}'])
                        A('act', lambda e, b=b, R=R, n=n: e.activation(out=gates[0:R, n, :], in_=ps[b][0:R, 0:48], func=AF.Sigmoid), r=[f'ps{b}'], w=['gates'])
                    P.emit()
            if upto >= 2:
                with ExitStack() as st:
                    TG = [(0, 512), (512, 512), (1024, NA - 1024)]
                    wt = [sb(st, f"wc{i}", [128, NKC, 128], BF16) for i in range(6)]
                    gbs = sb(st, "gbs", [128, NA])
                    gcs = [sb(st, f"gcs{i}", [128, 512]) for i in range(2)]
                    uA = sb(st, "uA", [128, NA])
                    uext = sb(st, "uext", [128, HALF + 2 + 96])
                    acc = sb(st, "acc", [128, NOUT])
                    sqt = sb(st, "sqt", [128, NOUT])
                    sqacc = sb(st, "sqacc", [128, NOUT])
                    cgt = [sb(st, f"cgt{i}", [128, NOUT], BF16) for i in range(2)]
                    ucol = sb(st, "ucol", [128, 16, 34])
                    A('pool', lambda e: e.memset(sqacc[:], 0.0), w=['sqacc'])
                    n = 0
                    for j in range(16):
                        sl = []
                        for r3 in range(3):
                            s = (j * 3 + r3) % 6
                            sl.append(s)
                            col = r3 * 2048 + j * 128
                            A('pool', lambda e, s=s, col=col: e.dma_start(out=wt[s][:], in_=w_in[:, col:col + 128].rearrange("(c p) n -> p c n", p=128)), w=[f'wc{s}'], dma=True)
                        for (t0, tn) in TG:
                            b0 = (n % 2) * 3
                            k2 = n % 2
                            n += 1
                            for r3 in range(3):
                                for c in range(NKC):
                                    A('pe', lambda e, b=b0 + r3, s=sl[r3], c=c, t0=t0, tn=tn: e.matmul(ps[b][:, 0:tn], lhsT=wt[s][:, c, :], rhs=xnT[:, c, t0:t0 + tn], start=(c == 0), stop=(c == NKC - 1)),
                                      r=['xnT', f'wc{sl[r3]}'], w=[f'ps{b0 + r3}'])
                            A('act', lambda e, b0=b0, t0=t0, tn=tn: e.copy(gbs[:, t0:t0 + tn], ps[b0][:, 0:tn]), r=[f'ps{b0}'], w=['gbs'])
                            A('act', lambda e, b0=b0, k2=k2, tn=tn: e.copy(gcs[k2][:, 0:tn], ps[b0 + 1][:, 0:tn]), r=[f'ps{b0 + 1}'], w=[f'gcs{k2}'])
                            A('dve', lambda e, b0=b0, k2=k2, t0=t0, tn=tn: e.tensor_tensor(out=uA[:, t0:t0 + tn], in0=ps[b0 + 2][:, 0:tn], in1=gcs[k2][:, 0:tn], op=ALU.mult),
                              r=[f'ps{b0 + 2}', f'gcs{k2}'], w=['uA'])
                        A('act', lambda e: e.copy(uext[:, 0:2], uA[:, NA - 2:NA]), r=['uA'], w=['uext'])
                        A('act', lambda e: e.copy(uext[:, 2:HALF + 2], uA[:, 0:HALF]), r=['uA'], w=['uext'])
                        ue_s = uext[:, HALF + 2:HALF + 98].rearrange("p (n t) -> p n t", t=6)
                        A('pool', lambda e, j=j, ue_s=ue_s: e.tensor_copy(ue_s[:, :, 0:2], scT[:].rearrange("p (n a c) -> p n a c", n=NS, a=2)[:, :, :, j]), r=['scT'], w=['uext'])
                        A('pool', lambda e, ue_s=ue_s: e.tensor_copy(ue_s[:, :, 2:6], uA[:, HALF:HALF + NST].rearrange("p (n t) -> p n t", t=4)), r=['uA'], w=['uext'])
                        acc_s = acc[:, HALF:NOUT].rearrange("p (n t) -> p n t", t=4)
                        w0, w1_, w2_ = cB[:, j:j + 1], cB[:, 16 + j:17 + j], cB[:, 32 + j:33 + j]
                        A('dve', lambda e, w2_=w2_: e.tensor_scalar(acc[:, 0:HALF], uext[:, 2:HALF + 2], w2_, None, op0=ALU.mult), r=['uext', 'cB'], w=['acc'])
                        A('dve', lambda e, w1_=w1_: e.scalar_tensor_tensor(out=acc[:, 0:HALF], in0=uext[:, 1:HALF + 1], scalar=w1_, in1=acc[:, 0:HALF], op0=ALU.mult, op1=ALU.add), r=['uext', 'cB', 'acc'], w=['acc'])
                        A('dve', lambda e, w0=w0: e.scalar_tensor_tensor(out=acc[:, 0:HALF], in0=uext[:, 0:HALF], scalar=w0, in1=acc[:, 0:HALF], op0=ALU.mult, op1=ALU.add), r=['uext', 'cB', 'acc'], w=['acc'])
                        A('dve', lambda e, w2_=w2_, ue_s=ue_s, acc_s=acc_s: e.tensor_scalar(acc_s, ue_s[:, :, 2:6], w2_, None, op0=ALU.mult), r=['uext', 'cB'], w=['acc_s'])
                        A('dve', lambda e, w1_=w1_, ue_s=ue_s, acc_s=acc_s: e.scalar_tensor_tensor(out=acc_s, in0=ue_s[:, :, 1:5], scalar=w1_, in1=acc_s, op0=ALU.mult, op1=ALU.add), r=['uext', 'cB', 'acc_s'], w=['acc_s'])
                        A('dve', lambda e, w0=w0, ue_s=ue_s, acc_s=acc_s: e.scalar_tensor_tensor(out=acc_s, in0=ue_s[:, :, 0:4], scalar=w0, in1=acc_s, op0=ALU.mult, op1=ALU.add), r=['uext', 'cB', 'acc_s'], w=['acc_s'])
                        A('dve', lambda e: e.tensor_tensor(out=acc[:], in0=acc[:], in1=gbs[:, 0:NOUT], op=ALU.mult), r=['acc', 'acc_s', 'gbs'], w=['acc', 'acc_s'])
                        A('pool', lambda e: e.tensor_tensor(out=sqt[:], in0=acc[:], in1=acc[:], op=ALU.mult), r=['acc', 'acc_s'], w=['sqt'])
                        A('pool', lambda e: e.tensor_tensor(out=sqacc[:], in0=sqacc[:], in1=sqt[:], op=ALU.add), r=['sqt', 'sqacc'], w=['sqacc'])
                        k2 = j % 2
                        A('act', lambda e, k2=k2, j=j: e.activation(out=cgt[k2][:], in_=acc[:], func=AF.Copy, scale=cA[:, 32 + j:33 + j]), r=['acc', 'acc_s', 'cA'], w=[f'cgt{k2}'])
                        A('sp', lambda e, k2=k2, j=j: e.dma_start(out=convT_s[j], in_=cgt[k2][:]), r=[f'cgt{k2}'], w=['convT_s'], dma=True)
                        A('pool', lambda e, j=j: e.tensor_copy(ucol[:, j, 0:2], uA[:, HALF - 2:HALF]), r=['uA'], w=['ucol'])
                        A('pool', lambda e, j=j: e.tensor_copy(ucol[:, j, 2:34].rearrange("p (n t) -> p n t", t=2), uA[:, HALF:HALF + NST].rearrange("p (n t) -> p n t", t=4)[:, :, 2:4]), r=['uA'], w=['ucol'])
                    A('sp', lambda e: e.dma_start(out=conv_o.rearrange("j p x -> p j x"), in_=ucol[:]), r=['ucol'], w=['conv_o'], dma=True)
                    for n, (ti, r0, R, c0) in enumerate(TILES_A):
                        Ro = min(R, 128 if ti < 16 else NST)
                        A('pe', lambda e, n=n, Ro=Ro, c0=c0: e.matmul(ps[6][0:Ro, 16 * n:16 * n + 2], lhsT=sqacc[:, c0:c0 + Ro], rhs=ones_f[:, 0:2], start=True, stop=True), r=['sqacc', 'ones_f'], w=['ps6'])
                    A('dve', lambda e: e.memset(inv_c[:], 1.0), w=['inv_c'])
                    ssv = ps[6][:, 0:144].rearrange("p (n x) -> p n x", x=16)
                    A('act', lambda e, ssv=ssv: e.activation(out=inv_c[:, 0:8], in_=ssv[:, 0:8, 0], func=AF.Sqrt, bias=epsc[:], scale=1.0 / 2048), r=['ps6', 'epsc', 'inv_c'], w=['inv_c'])
                    A('act', lambda e, ssv=ssv: e.activation(out=inv_c[0:NST, 8:9], in_=ssv[0:NST, 8:9, 0], func=AF.Sqrt, bias=epsc[0:NST, :], scale=1.0 / 2048), r=['ps6', 'epsc', 'inv_c'], w=['inv_c'])
                    A('dve', lambda e: e.reciprocal(inv_c[:], inv_c[:]), r=['inv_c'], w=['inv_c'])
                    P.emit()
            if upto >= 3:
                xnorm(TILES_B)
                with ExitStack() as st:
                    kv_project(TILES_B, st)
                    P.emit()

        attnT = sb(st_mid, "attnT", [128, 16, NOUT], BF16)
        smdbg = sb(st_mid, "smdbg", [128, 256])
        dbg2 = sb(st_mid, "dbg2", [128, 8])
        def cmp_factory(st, k_cT, VA):
            w1b = sb(st, "w1b", [128, 64, 128], BF16)
            w2b = sb(st, "w2b", [128, 2, 128], BF16)
            peTb = sb(st, "peTb", [128, 66], BF16)
            pe_bias = sb(st, "pe_bias", [128, 2])
            kvT = sb(st, "kvT", [128, 8, SEQ], BF16)
            kvc_t = [sb(st, f"kvc_t{i}", [128, 1024], BF16) for i in range(3)]
            c1s = sb(st, "c1s", [128, 4, 128])
            hp = sb(st, "hp", [128, 4, 128])
            hid = [sb(st, f"hid{i}", [128, 4, 128], BF16) for i in range(2)]
            sqk = sb(st, "sqk", [128, 512])
            rk = sb(st, "rk", [128, 512])
            A('pool', lambda e: e.dma_start(out=w1b[:], in_=phi_w1.rearrange("v l d e -> d (v l) e")), w=['w1b'], dma=True)
            A('pool', lambda e: e.dma_start(out=w2b[:], in_=phi_w2.rearrange("v e d -> e v d")), w=['w2b'], dma=True)
            pprod = sb(st, "pprod", [128, 32, 128])
            pred = sb(st, "pred", [128, 128])
            for v in range(2):
                A('dve', lambda e, v=v: e.tensor_tensor(out=pprod[:], in0=w1b[:, v * 32:(v + 1) * 32, :], in1=bc(cB[:, 64 + v * 32:64 + (v + 1) * 32].unsqueeze(2), [128, 32, 128]), op=ALU.mult),
                  r=['w1b', 'cB'], w=['pprod'])
                A('dve', lambda e: e.tensor_reduce(out=pred[:], in_=pprod[:].rearrange("p l e -> p e l"), axis=AX.X, op=ALU.add), r=['pprod'], w=['pred'])
                A('pe', lambda e, v=v: e.matmul(ps[v][:, 0:2], lhsT=pred[:], rhs=ones_f[:, 0:2], start=True, stop=True), r=['pred', 'ones_f'], w=[f'ps{v}'])
                A('dve', lambda e, v=v: e.tensor_copy(pe_bias[:, v:v + 1], ps[v][:, 0:1]), r=[f'ps{v}'], w=['pe_bias'])

            def cmp_prep(load_tile, ntiles=16):
                for rt in range(ntiles):
                    s3 = rt % 3
                    load_tile(rt, kvc_t[s3], f'kvc_t{s3}')
                    b = rt % 2
                    for vj in range(8):
                        A('pe', lambda e, b=b, s3=s3, vj=vj: e.transpose(psb[b][:, vj * 128:(vj + 1) * 128], kvc_t[s3][:, vj * 128:(vj + 1) * 128], ident[:]),
                          r=[f'kvc_t{s3}', 'ident'], w=[f'ps{b}'])
                    eng = 'act' if rt % 2 == 0 else 'dve'
                    if eng == 'act':
                        A('act', lambda e, b=b, rt=rt: e.copy(kvT[:, :, rt * 128:(rt + 1) * 128], psb[b].rearrange("p (a t) -> p a t", a=8)), r=[f'ps{b}'], w=['kvT'])
                    else:
                        A('dve', lambda e, b=b, rt=rt: e.tensor_copy(kvT[:, :, rt * 128:(rt + 1) * 128], psb[b].rearrange("p (a t) -> p a t", a=8)), r=[f'ps{b}'], w=['kvT'])
                for v in range(2):
                    for r2 in range(2):
                        b = 4 + v * 2 + r2
                        for s_ in range(16):
                            A('pe', lambda e, b=b, v=v, r2=r2, s_=s_: e.matmul(ps[b][:].rearrange("p (j c) -> p j c", j=4), lhsT=w1b[:, v * 32 + r2 * 16 + s_, :],
                                                                              rhs=kvT[:, v * 4:(v + 1) * 4, :].rearrange("p j (c s) -> p j c s", s=16)[:, :, :, s_], start=(s_ == 0), stop=(s_ == 15)),
                              r=['kvT', 'w1b'], w=[f'ps{b}'])
                for v in range(2):
                    b0, b1 = 4 + v * 2, 5 + v * 2
                    p0v = ps[b0][:].rearrange("p (j c) -> p j c", j=4)
                    A('act', lambda e, b1=b1, v=v: e.activation(out=c1s[:].rearrange("p j c -> p (j c)"), in_=ps[b1][:], func=AF.Identity, bias=pe_bias[:, v:v + 1], scale=1.0),
                      r=[f'ps{b1}', 'pe_bias'], w=['c1s'])
                    A('dve', lambda e, p0v=p0v: e.tensor_tensor(out=hp[:, :, 0:127], in0=p0v[:, :, 0:127], in1=c1s[:, :, 1:128], op=ALU.add), r=[f'ps{b0}', 'c1s'], w=['hp'])
                    A('dve', lambda e, p0v=p0v: e.tensor_tensor(out=hp[:, :, 127:128], in0=p0v[:, :, 127:128], in1=c1s[:, :, 0:1], op=ALU.add), r=[f'ps{b0}', 'c1s', 'hp'], w=['hp'])
                    A('act', lambda e, v=v: e.activation(out=hid[v][:], in_=hp[:], func=AF.Gelu_apprx_tanh), r=['hp'], w=[f'hid{v}'])
                A('pe', lambda e: e.matmul(ps[0][:], lhsT=w2b[:, 0, :], rhs=hid[0][:].rearrange("p j c -> p (j c)"), start=True, stop=True), r=['w2b', 'hid0'], w=['ps0'])
                A('act', lambda e: e.activation(out=sqk[:], in_=ps[0][:], func=AF.Square), r=['ps0'], w=['sqk'])
                A('pe', lambda e: e.matmul(ps[1][:], lhsT=ones_f[:], rhs=sqk[:], start=True, stop=True), r=['sqk', 'ones_f'], w=['ps1'])
                A('act', lambda e: e.activation(out=rk[:], in_=ps[1][:], func=AF.Sqrt, bias=epsc[:], scale=1.0 / HD), r=['ps1', 'epsc'], w=['rk'])
                A('dve', lambda e: e.reciprocal(rk[:], rk[:]), r=['rk'], w=['rk'])
                A('dve', lambda e: e.scalar_tensor_tensor(out=k_cT[:].rearrange("p j c -> p (j c)"), in0=ps[0][:], scalar=cA[:, 96:97], in1=rk[:], op0=ALU.mult, op1=ALU.mult),
                  r=['ps0', 'rk', 'cA'], w=['k_cT'])
                for j in range(4):
                    A('pe', lambda e, j=j: e.matmul(ps[2][:, j * 128:(j + 1) * 128], lhsT=hid[1][:, j, :], rhs=w2b[:, 1, :], start=True, stop=True), r=['w2b', 'hid1'], w=['ps2'])
                A('act', lambda e: e.copy(VA[:].rearrange("p j d -> p (j d)"), ps[2][:]), r=['ps2'], w=['VA'])

            return cmp_prep

        if upto >= 4:
            with ExitStack() as st:
                cmpT = sb(st, "cmpT", [128, HALF], BF16)
                MZ = sb(st, "MZ", [128, 33], BF16)
                madd = sb(st, "madd", [128, 8, 32])
                mcblk = sb(st, "mcblk", [128, 8, 32])
                mE = sb(st, "mE", [32, SEQ], BF16)
                tri = sb(st, "tri", [128, 256], BF16)
                for (dst, src, k) in ((cmpT, m_cmpT, 'cmpT'), (MZ, m_MZ, 'MZ'), (madd, m_add, 'madd'), (mcblk, m_cblk, 'mcblk'), (mE, m_E, 'mE'), (tri, m_tri, 'tri')):
                    A('sp', lambda e, dst=dst, src=src: e.dma_start(out=dst[:], in_=src), w=[k], dma=True)
                k_cT = sb(st, "k_cT", [128, 4, 128], BF16)
                VA = sb(st, "VA", [128, 4, 128], BF16)

                with ExitStack() as stc:
                    def load_prompt_cmp(rt, dst, key):
                        A('pool', lambda e, rt=rt, dst=dst: e.dma_start(out=dst[:], in_=kv_o[0][rt * 128:(rt + 1) * 128, :]), r=['kvo0'], w=[key], dma=True)
                    cmp_factory(stc, k_cT, VA)(load_prompt_cmp)
                    P.emit()

                kT = [sb(st, f"kT{i}", [128, 4, SEQ], BF16) for i in range(2)]
                Vx = [sb(st, f"Vx{i}", [128, 16, 4, 129], BF16) for i in range(2)]
                kst = [sb(st, f"kst{i}", [128, 512], BF16) for i in range(3)]
                n = 0
                for bi in range(2):
                    A('pool', lambda e, bi=bi: e.memset(Vx[bi][:], 1.0), w=[f'Vx{bi}'])
                    for rt in range(16):
                        A('pool', lambda e, bi=bi, rt=rt: e.dma_start(out=Vx[bi][:, rt, :, 0:128], in_=kv_o[1 + bi][rt * 128:(rt + 1) * 128, 512:1024].rearrange("p (j d) -> p j d", j=4)),
                          r=[f'kvo{1 + bi}'], w=[f'Vx{bi}'], dma=True)
                        s3 = n % 3
                        b = n % 2
                        n += 1
                        A('pool', lambda e, bi=bi, rt=rt, s3=s3: e.dma_start(out=kst[s3][:], in_=kv_o[1 + bi][rt * 128:(rt + 1) * 128, 0:512]), r=[f'kvo{1 + bi}'], w=[f'kst{s3}'], dma=True)
                        for j in range(4):
                            A('pe', lambda e, b=b, s3=s3, j=j: e.transpose(psb[b][:, j * 128:(j + 1) * 128], kst[s3][:, j * 128:(j + 1) * 128], ident[:]), r=[f'kst{s3}', 'ident'], w=[f'ps{b}'])
                        if rt % 2 == 0:
                            A('act', lambda e, b=b, bi=bi, rt=rt: e.copy(kT[bi][:, :, rt * 128:(rt + 1) * 128], psb[b][:, 0:512].rearrange("p (a t) -> p a t", a=4)), r=[f'ps{b}'], w=[f'kT{bi}'])
                        else:
                            A('dve', lambda e, b=b, bi=bi, rt=rt: e.tensor_copy(kT[bi][:, :, rt * 128:(rt + 1) * 128], psb[b][:, 0:512].rearrange("p (a t) -> p a t", a=4)), r=[f'ps{b}'], w=[f'kT{bi}'])

                etmp = sb(st, "etmp", [128, 4, 128], BF16)
                ETm = sb(st, "ETm", [128, 4, 128], BF16)
                pexp = [sb(st, f"pexp{i}", [128, 4, 128], BF16) for i in range(3)]
                PT = [sb(st, f"PT{i}", [128, 4, 128], BF16) for i in range(3)]
                mk = [sb(st, f"mk{i}", [128, 128], BF16) for i in range(2)]
                attn = [sb(st, f"attn{i}", [128, 16, 128]) for i in range(2)]
                attb = sb(st, "attb", [128, 16, 128], BF16)
                sm = sb(st, "sm", [128, 256])
                tmp3 = sb(st, "tmp3", [128, 4, 32])
                selT_b = sb(st, "selT_b", [32, 128], BF16)
                assq = sb(st, "assq", [128, 16])
                junk2 = sb(st, "junk2", [128, 2048], BF16)
                triL, triU = tri[:, 0:128], tri[:, 128:256]
                nps = [0]

                def sbank():
                    nps[0] += 1
                    return nps[0] % 2

                for i in range(8):
                    at = attn[i % 2]
                    ak = f'attn{i % 2}'
                    for j in range(4):
                        rhs_q = qT[:, 4 * j:4 * j + 4, 128 * i:128 * (i + 1)]
                        b = sbank()
                        A('pe', lambda e, b=b, j=j, rhs_q=rhs_q: e.matmul(ps[b][:].rearrange("p (g q) -> p g q", g=4), lhsT=k_cT[:, j, :], rhs=rhs_q, start=True, stop=True), r=['k_cT', 'qT'], w=[f'ps{b}'])
                        A('act', lambda e, b=b: e.activation(out=etmp[:].rearrange("p g q -> p (g q)"), in_=ps[b][:], func=AF.Exp, scale=SCALE), r=[f'ps{b}'], w=['etmp'])
                        for g in range(4):
                            A('dve', lambda e, i=i, g=g: e.tensor_tensor(out=ETm[:, g, :], in0=etmp[:, g, :], in1=cmpT[:, 128 * i:128 * (i + 1)], op=ALU.mult), r=['etmp', 'cmpT'], w=['ETm'])
                        for g in range(4):
                            A('pe', lambda e, g=g, j=j: e.matmul(ps[3][:, g * 128:(g + 1) * 128], lhsT=ETm[:, g, :], rhs=VA[:, j, :], start=True, stop=True), r=['ETm', 'VA'], w=['ps3'])
                            A('pe', lambda e, g=g: e.matmul(ps[2][:, g * 64:g * 64 + 33], lhsT=ETm[:, g, :], rhs=MZ[:], start=True, stop=True), r=['ETm', 'MZ'], w=['ps2'])
                        iz = ps[2][:, 0:256].rearrange("p (g b) -> p g b", g=4)
                        A('dve', lambda e, iz=iz: e.tensor_scalar(sm[:, 0:4], iz[:, :, 32], 1e-30, None, op0=ALU.max), r=['ps2'], w=['sm_rz'])
                        A('dve', lambda e: e.reciprocal(sm[:, 0:4], sm[:, 0:4]), r=['sm_rz'], w=['sm_rz'])
                        A('dve', lambda e, i=i, j=j: e.tensor_tensor(out=sm[:, 4:8], in0=gates[:, i, 4 * j:4 * j + 4], in1=sm[:, 0:4], op=ALU.mult), r=['sm_rz', 'gates'], w=['sm_cf'])
                        for g in range(4):
                            A('dve', lambda e, at=at, j=j, g=g: e.tensor_scalar(at[:, 4 * j + g, :], ps[3][:, g * 128:(g + 1) * 128], sm[:, 4 + g:5 + g], None, op0=ALU.mult),
                              r=['ps3', 'sm_cf'], w=[ak])
                        if DBG_BR not in (-1, 0):
                            A('dve', lambda e, at=at, j=j: e.memset(at[:, 4 * j:4 * j + 4, :], 0.0), w=[ak])
                        for g in range(4):
                            A('dve', lambda e, iz=iz, g=g: e.tensor_scalar(tmp3[:, g, :], iz[:, g, 0:32], sm[:, g:g + 1], None, op0=ALU.mult), r=['ps2', 'sm_rz'], w=['tmp3'])
                        A('dve', lambda e: e.tensor_reduce(out=sm[:, 8:40], in_=tmp3[:].rearrange("p g b -> p b g"), axis=AX.X, op=ALU.add), r=['tmp3'], w=['sm_imp'])
                        A('dve', lambda e, i=i: e.tensor_tensor(out=sm[:, 8:40], in0=sm[:, 8:40], in1=madd[:, i, :], op=ALU.add), r=['sm_imp', 'madd'], w=['sm_imp'])
                        A('dve', lambda e: e.max(out=sm[:, 40:48], in_=sm[:, 8:40]), r=['sm_imp'], w=['sm_mx'])
                        A('dve', lambda e: e.tensor_scalar(sm[:, 48:80], sm[:, 8:40], sm[:, 47:48], None, op0=ALU.is_ge), r=['sm_imp', 'sm_mx'], w=['sm_sel'])
                        A('dve', lambda e, i=i: e.tensor_tensor(out=sm[:, 48:80], in0=sm[:, 48:80], in1=mcblk[:, i, :], op=ALU.mult), r=['sm_sel', 'mcblk'], w=['sm_sel'])
                        A('pe', lambda e: e.transpose(ps[2][0:32, 256:384], sm[:, 48:80], identf[:]), r=['sm_sel', 'identf'], w=['ps2'])
                        A('act', lambda e: e.copy(selT_b[:], ps[2][0:32, 256:384]), r=['ps2'], w=['selT_b'])
                        kts = list(range(0, i + 1)) + list(range(8, 16))
                        for n_, kt in enumerate(kts):
                            b = sbank()
                            s3 = n_ % 3
                            A('pe', lambda e, b=b, j=j, kt=kt, rhs_q=rhs_q: e.matmul(ps[b][:].rearrange("p (g q) -> p g q", g=4), lhsT=kT[0][:, j, kt * 128:(kt + 1) * 128], rhs=rhs_q, start=True, stop=True),
                              r=['kT0', 'qT'], w=[f'ps{b}'])
                            mb = 2 + n_ % 2
                            A('pe', lambda e, kt=kt, mb=mb: e.matmul(ps[mb][:, 384:512], lhsT=mE[0:32, kt * 128:(kt + 1) * 128], rhs=selT_b[:], start=True, stop=True), r=['mE', 'selT_b'], w=[f'ps{mb}'])
                            A('act', lambda e, b=b, s3=s3: e.activation(out=pexp[s3][:].rearrange("p g q -> p (g q)"), in_=ps[b][:], func=AF.Exp, scale=SCALE), r=[f'ps{b}'], w=[f'pexp{s3}'])
                            if kt == i:
                                A('dve', lambda e, mb=mb: e.tensor_tensor(out=mk[0][:], in0=ps[mb][:, 384:512], in1=triL, op=ALU.mult), r=[f'ps{mb}', 'tri'], w=['mk0'])
                                for g in range(4):
                                    A('dve', lambda e, s3=s3, g=g: e.tensor_tensor(out=PT[s3][:, g, :], in0=pexp[s3][:, g, :], in1=mk[0][:], op=ALU.mult), r=[f'pexp{s3}', 'mk0'], w=[f'PT{s3}'])
                            else:
                                A('act', lambda e, mb=mb: e.copy(mk[1][:], ps[mb][:, 384:512]), r=[f'ps{mb}'], w=['mk1'])
                                for g in range(4):
                                    A('dve', lambda e, s3=s3, g=g: e.tensor_tensor(out=PT[s3][:, g, :], in0=pexp[s3][:, g, :], in1=mk[1][:], op=ALU.mult), r=[f'pexp{s3}', 'mk1'], w=[f'PT{s3}'])
                            for g in range(4):
                                o_ap = ps[4 + g][:, 0:129]
                                A('pe', lambda e, g=g, s3=s3, kt=kt, j=j, o_ap=o_ap, n_=n_, last=(n_ == len(kts) - 1): e.matmul(o_ap, lhsT=PT[s3][:, g, :], rhs=Vx[0][:, kt, j, :], start=(n_ == 0), stop=last),
                                  r=[f'PT{s3}', 'Vx0'], w=[f'ps{4 + g}'])

                        def finish(bA, bB, goff, at=at, ak=ak, i=i, j=j):
                            for g in range(4):
                                A('dve', lambda e, g=g: e.tensor_scalar(sm[:, 80 + g:81 + g], ps[4 + g][:, 128:129], 1e-30, None, op0=ALU.max), r=[f'ps{4 + g}'], w=[f'sm_z{g}'])
                            A('dve', lambda e: e.reciprocal(sm[:, 80:84], sm[:, 80:84]), r=[f'sm_z{g}' for g in range(4)], w=[f'sm_z{g}' for g in range(4)])
                            A('dve', lambda e: e.tensor_tensor(out=sm[:, 84:88], in0=gates[:, i, goff + 4 * j:goff + 4 * j + 4], in1=sm[:, 80:84], op=ALU.mult), r=[f'sm_z{g}' for g in range(4)] + ['gates'], w=['sm_cf2'])
                            for g in range(4):
                                if DBG_BR != -1 and DBG_BR != goff // 16:
                                    continue
                                o_ap = ps[4 + g][:, 0:128]
                                A('dve', lambda e, g=g, o_ap=o_ap: e.scalar_tensor_tensor(out=at[:, 4 * j + g, :], in0=o_ap, scalar=sm[:, 84 + g:85 + g], in1=at[:, 4 * j + g, :], op0=ALU.mult, op1=ALU.add),
                                  r=[f'ps{4 + g}', 'sm_cf2', ak], w=[ak])
                        finish(4, 5, 16)
                        for r5 in range(5):
                            k = i - 4 + r5
                            kt = k if k >= 0 else 16 + k
                            b = sbank()
                            s3 = r5 % 3
                            A('pe', lambda e, b=b, j=j, kt=kt, rhs_q=rhs_q: e.matmul(ps[b][:].rearrange("p (g q) -> p g q", g=4), lhsT=kT[1][:, j, kt * 128:(kt + 1) * 128], rhs=rhs_q, start=True, stop=True),
                              r=['kT1', 'qT'], w=[f'ps{b}'])
                            A('act', lambda e, b=b, s3=s3: e.activation(out=pexp[s3][:].rearrange("p g q -> p (g q)"), in_=ps[b][:], func=AF.Exp, scale=SCALE), r=[f'ps{b}'], w=[f'pexp{s3}'])
                            msk = triU if r5 == 0 else (triL if r5 == 4 else None)
                            src = pexp[s3]
                            srck = f'pexp{s3}'
                            if k < 0 and msk is not None:
                                for g in range(4):
                                    A('dve', lambda e, s3=s3, msk=msk, g=g: e.scalar_tensor_tensor(out=PT[s3][:, g, :], in0=pexp[s3][:, g, :], scalar=flag[:, 0:1], in1=msk, op0=ALU.mult, op1=ALU.mult),
                                      r=[f'pexp{s3}', 'flag', 'tri'], w=[f'PT{s3}'])
                                src, srck = PT[s3], f'PT{s3}'
                            elif k < 0:
                                A('dve', lambda e, s3=s3: e.tensor_scalar(PT[s3][:], pexp[s3][:], flag[:, 0:1], None, op0=ALU.mult), r=[f'pexp{s3}', 'flag'], w=[f'PT{s3}'])
                                src, srck = PT[s3], f'PT{s3}'
                            elif msk is not None:
                                for g in range(4):
                                    A('dve', lambda e, s3=s3, msk=msk, g=g: e.tensor_tensor(out=PT[s3][:, g, :], in0=pexp[s3][:, g, :], in1=msk, op=ALU.mult), r=[f'pexp{s3}', 'tri'], w=[f'PT{s3}'])
                                src, srck = PT[s3], f'PT{s3}'
                            for g in range(4):
                                o_ap = ps[4 + g][:, 0:129]
                                A('pe', lambda e, g=g, src=src, kt=kt, j=j, o_ap=o_ap, r5=r5: e.matmul(o_ap, lhsT=src[:, g, :], rhs=Vx[1][:, kt, j, :], start=(r5 == 0), stop=(r5 == 4)),
                                  r=[srck, 'Vx1'], w=[f'ps{4 + g}'])
                        finish(6, 7, 32)
                    if i == 7:
                        A('dve', lambda e: e.tensor_copy(smdbg[:], sm[:]), r=['sm_rz', 'sm_cf', 'sm_imp', 'sm_mx', 'sm_sel', 'sm_cf2'], w=['smdbg'])
                    A('dve', lambda e, i=i: e.memset(assq[:, i:i + 1], 0.0), w=[f'assq{i}'])
                    A('act', lambda e, at=at, i=i: e.activation(out=junk2[:], in_=at[:].rearrange("p h d -> p (h d)"), func=AF.Square, accum_out=assq[:, i:i + 1]), r=[ak], w=['junk2', f'assq{i}'])
                    A('act', lambda e, i=i: e.activation(out=inv_a[:, i:i + 1], in_=assq[:, i:i + 1], func=AF.Sqrt, bias=epsc[:], scale=1.0 / 2048), r=[f'assq{i}', 'epsc'], w=['inv_a'])
                    A('dve', lambda e, i=i: e.reciprocal(inv_a[:, i:i + 1], inv_a[:, i:i + 1]), r=['inv_a'], w=['inv_a'])
                    A('act', lambda e, at=at: e.copy(attb[:], at[:]), r=[ak], w=['attb'])
                    for g2 in range(2):
                        b = sbank()
                        for c in range(8):
                            A('pe', lambda e, b=b, g2=g2, c=c: e.transpose(psb[b][:, c * 128:(c + 1) * 128], attb[:, g2 * 8 + c, :], ident[:]), r=['attb', 'ident'], w=[f'ps{b}'])
                        A('dve', lambda e, b=b, g2=g2, i=i: e.tensor_tensor(out=attnT[:, g2 * 8:(g2 + 1) * 8, 128 * i:128 * (i + 1)], in0=psb[b].rearrange("p (c t) -> p c t", c=8),
                                                                         in1=bc(goutT[:, 16 + g2 * 8:16 + (g2 + 1) * 8].unsqueeze(2), [128, 8, 128]), op=ALU.mult), r=[f'ps{b}', 'cA'], w=['attnT'])
                P.emit()

        if upto >= 5:
            SC0 = HALF
            with ExitStack() as st:
                sMZ = sb(st, "sMZ", [128, 34], BF16)
                misc = sb(st, "misc", [128, 64])
                sadd = sb(st, "sadd", [64, 40])
                selN = sb(st, "selN", [64, NS, 128], BF16)
                idx = sb(st, "idx", [128, 256], I32)
                idxf = sb(st, "idxf", [128, 256])
                iot = sb(st, "iot", [128, 1])
                Es = sb(st, "Es", [128, 16, NS, 4], BF16)
                OTb = [sb(st, f"OTb{i}", [128, 16, NS, 4]) for i in range(3)]
                Dm = sb(st, "Dm", [64, 4, 33, 4], BF16)
                knT = [sb(st, f"knT{i}", [128, 4, NST], BF16) for i in range(2)]
                for (dst, src, k) in ((sMZ, s_MZ, 'sMZ'), (misc, s_misc, 'misc'), (sadd, s_add, 'sadd')):
                    A('sp', lambda e, dst=dst, src=src: e.dma_start(out=dst[:], in_=src), w=[k], dma=True)
                A('sp', lambda e: e.dma_start(out=selN[:].rearrange("p n k -> p (n k)"), in_=s_selN), w=['selN'], dma=True)
                A('sp', lambda e: e.dma_start(out=idx[:], in_=page_table.partition_broadcast(128)), w=['idx'], dma=True)
                A('pool', lambda e: e.iota(iot[:], pattern=[[0, 1]], base=0, channel_multiplier=1, allow_small_or_imprecise_dtypes=True), w=['iot'])
                A('dve', lambda e: e.tensor_copy(idxf[:], idx[:]), r=['idx'], w=['idxf'])
                A('dve', lambda e: e.tensor_scalar(idxf[:], idxf[:], 128.0, iot[:, 0:1], op0=ALU.mult, op1=ALU.add), r=['idxf', 'iot'], w=['idxf'])
                A('dve', lambda e: e.tensor_copy(idx[:], idxf[:]), r=['idxf'], w=['idx'])
                hb, hb0, cval = misc[:, 0:1], misc[:, 1:2], misc[:, 2:3]
                I4 = misc[0:64, 4:8]
                cnew = misc[0:4, 8:24]
                mwin = misc[:, 24:40]
                with ExitStack() as stn:
                    nrow = sb(stn, "nrow", [64, 512], BF16)
                    for bi in range(2):
                        A('pool', lambda e, bi=bi: e.dma_start(out=nrow[:], in_=kv_o[1 + bi][SEQ:SEQ + NST, 0:512]), r=[f'kvo{1 + bi}'], w=['nrow'], dma=True)
                        for j in range(4):
                            A('pe', lambda e, j=j: e.transpose(psb[0][:, j * 64:(j + 1) * 64], nrow[:, j * 128:(j + 1) * 128], ident[0:64, 0:64]), r=['nrow', 'ident'], w=['ps0'])
                        A('act', lambda e, bi=bi: e.copy(knT[bi][:].rearrange("p j n -> p (j n)"), psb[0][:, 0:256]), r=['ps0'], w=[f'knT{bi}'])
                    k_cT = sb(stn, "sk_cT", [128, 4, 128], BF16)
                    VA = sb(stn, "sVA", [128, 4, 128], BF16)
                    es_t = sb(stn, "es_t", [128, 64], BF16)
                    zr = sb(stn, "zr", [128, 64])
                    cmp_prep = cmp_factory(stn, k_cT, VA)
                    for n in range(NS):
                        def load_cmp(rt, dst, key, n=n):
                            A('pool', lambda e, rt=rt, dst=dst, n=n: e.indirect_dma_start(out=dst[:], out_offset=None, in_=cache_cmp,
                                                                                      in_offset=bass.IndirectOffsetOnAxis(ap=idx[:, n * 16 + rt:n * 16 + rt + 1], axis=0)), r=['idx'], w=[key], dma=True)
                        cmp_prep(load_cmp)
                        qc = SC0 + 4 * n
                        for j in range(4):
                            A('pe', lambda e, j=j, qc=qc: e.matmul(ps[3][:, j * 16:(j + 1) * 16].rearrange("p (g t) -> p g t", g=4), lhsT=k_cT[:, j, :], rhs=qT[:, 4 * j:4 * j + 4, qc:qc + 4], start=True, stop=True),
                              r=['k_cT', 'qT'], w=['ps3'])
                        A('act', lambda e: e.activation(out=es_t[:], in_=ps[3][:, 0:64], func=AF.Exp, scale=SCALE), r=['ps3'], w=['es_t'])
                        A('dve', lambda e, n=n: e.tensor_scalar(Es[:, :, n, :], es_t[:].rearrange("p (a t) -> p a t", t=4), cval, None, op0=ALU.mult), r=['es_t', 'misc'], w=['Es'])
                        for j in range(4):
                            A('pe', lambda e, j=j, n=n: e.matmul(ps[3][:, 64 + j * 16:64 + (j + 1) * 16].rearrange("p (g t) -> p g t", g=4), lhsT=VA[:, j, :], rhs=Es[:, 4 * j:4 * j + 4, n, :], start=True, stop=True),
                              r=['VA', 'Es'], w=['ps3'])
                        A('pe', lambda e, n=n: e.matmul(ps[3][:, 128:192].rearrange("p (a t) -> p a t", t=4), lhsT=ones_b[:], rhs=Es[:, :, n, :], start=True, stop=True), r=['ones_b', 'Es'], w=['ps3'])
                        A('dve', lambda e: e.tensor_scalar(zr[:], ps[3][:, 128:192], 1e-30, None, op0=ALU.max), r=['ps3'], w=['zr'])
                        A('dve', lambda e: e.reciprocal(zr[:], zr[:]), r=['zr'], w=['zr'])
                        A('dve', lambda e, n=n: e.tensor_tensor(out=OTb[0][:, :, n, :], in0=ps[3][:, 64:128].rearrange("p (a t) -> p a t", t=4), in1=zr[:].rearrange("p (a t) -> p a t", t=4), op=ALU.mult),
                          r=['ps3', 'zr'], w=['OTb0'])
                    smp = sb(stn, "smp", [64, 512])
                    for jg in range(16):
                        bq = 4 + jg // 8
                        A('pe', lambda e, jg=jg, bq=bq: e.matmul(ps[bq][0:64, (jg % 8) * 64:(jg % 8) * 64 + 34], lhsT=Es[:, jg].rearrange("p n t -> p (n t)"), rhs=sMZ[:], start=True, stop=True), r=['Es', 'sMZ'], w=[f'ps{bq}'])
                    for j in range(4):
                        for g in range(4):
                            jg = j * 4 + g
                            izp = ps[4 + jg // 8][0:64, (jg % 8) * 64:(jg % 8) * 64 + 34]
                            A('dve', lambda e, izp=izp: e.tensor_scalar(smp[:, 0:1], izp[:, 33:34], 1e-30, None, op0=ALU.max), r=[f'ps{4 + jg // 8}'], w=['smp_z'])
                            A('dve', lambda e: e.reciprocal(smp[:, 0:1], smp[:, 0:1]), r=['smp_z'], w=['smp_z'])
                            if g == 0:
                                A('dve', lambda e, izp=izp: e.tensor_scalar(smp[:, 8:41], izp[:, 0:33], smp[:, 0:1], None, op0=ALU.mult), r=[f'ps{4 + jg // 8}', 'smp_z'], w=['smp_imp'])
                            else:
                                A('dve', lambda e, izp=izp: e.scalar_tensor_tensor(out=smp[:, 8:41], in0=izp[:, 0:33], scalar=smp[:, 0:1], in1=smp[:, 8:41], op0=ALU.mult, op1=ALU.add),
                                  r=[f'ps{4 + jg // 8}', 'smp_z', 'smp_imp'], w=['smp_imp'])
                        A('dve', lambda e: e.tensor_copy(smp[:, 48:88], sadd[:]), r=['sadd'], w=['smp_sc'])
                        A('dve', lambda e: e.tensor_tensor(out=smp[:, 48:81], in0=smp[:, 48:81], in1=smp[:, 8:41], op=ALU.add), r=['smp_sc', 'smp_imp'], w=['smp_sc'])
                        A('dve', lambda e: e.max(out=smp[:, 96:104], in_=smp[:, 48:88]), r=['smp_sc'], w=['smp_mx'])
                        A('dve', lambda e: e.tensor_scalar(smp[:, 112:145], smp[:, 48:81], smp[:, 103:104], None, op0=ALU.is_ge), r=['smp_sc', 'smp_mx'], w=['smp_sel'])
                        for t2 in range(4):
                            A('dve', lambda e, j=j, t2=t2: e.tensor_scalar(Dm[:, j, :, t2], smp[:, 112:145], I4[:, t2:t2 + 1], None, op0=ALU.mult), r=['smp_sel', 'misc'], w=['Dm'])
                    P.emit()
                with ExitStack() as stn:
                    pgs = [sb(stn, f"pgs{i}", [128, 1024], BF16) for i in range(3)]
                    kTs = sb(stn, "kTs", [128, 4, SEQ], BF16)
                    Vs = sb(stn, "Vs", [128, 16, 512], BF16)
                    kTw = sb(stn, "kTw", [128, 4, 512], BF16)
                    Vw = sb(stn, "Vw", [128, 4, 512], BF16)
                    vnew = [sb(stn, f"vnew{i}", [4, 512], BF16) for i in range(2)]
                    mks = sb(stn, "mks", [128, 16, 4], BF16)
                    pes = sb(stn, "pes", [128, 16, 4, 4], BF16)
                    Pm = sb(stn, "Pm", [128, 16, 4, 4], BF16)
                    pnw = sb(stn, "pnw", [4, 4, 4])
                    Pn = sb(stn, "Pn", [4, 4, 4], BF16)
                    Pnf = sb(stn, "Pnf", [4, 4, 4])
                    Psum = sb(stn, "Psum", [128, 16])
                    zr2 = sb(stn, "zr2", [128, 16])
                    osum = sb(stn, "osum", [128, 16])
                    mwb = sb(stn, "mwb", [128, 4, 4], BF16)
                    A('dve', lambda e: e.tensor_copy(mwb[:].rearrange("p a t -> p (a t)"), mwin), r=['misc'], w=['mwb'])
                    npg = [0]
                    for n in range(NS):
                        qc = SC0 + 4 * n
                        for rt in range(16):
                            s3 = npg[0] % 3
                            b = npg[0] % 2
                            npg[0] += 1
                            A('pool', lambda e, s3=s3, n=n, rt=rt: e.indirect_dma_start(out=pgs[s3][:], out_offset=None, in_=cache_sel,
                                                                                      in_offset=bass.IndirectOffsetOnAxis(ap=idx[:, n * 16 + rt:n * 16 + rt + 1], axis=0)), r=['idx'], w=[f'pgs{s3}'], dma=True)
                            for j in range(4):
                                A('pe', lambda e, b=b, s3=s3, j=j: e.transpose(psb[b][:, j * 128:(j + 1) * 128], pgs[s3][:, j * 128:(j + 1) * 128], ident[:]), r=[f'pgs{s3}', 'ident'], w=[f'ps{b}'])
                            A('act', lambda e, b=b, rt=rt: e.copy(kTs[:, :, rt * 128:(rt + 1) * 128], psb[b][:, 0:512].rearrange("p (a t) -> p a t", a=4)), r=[f'ps{b}'], w=['kTs'])
                            A('pool', lambda e, s3=s3, rt=rt: e.tensor_copy(Vs[:, rt, :], pgs[s3][:, 512:1024]), r=[f'pgs{s3}'], w=['Vs'])
                        for rt in range(4):
                            s3 = npg[0] % 3
                            b = npg[0] % 2
                            npg[0] += 1
                            A('pool', lambda e, s3=s3, n=n, rt=rt: e.dma_start(out=pgs[s3][:], in_=state_win[n, rt * 128:(rt + 1) * 128, :]), w=[f'pgs{s3}'], dma=True)
                            for j in range(4):
                                A('pe', lambda e, b=b, s3=s3, j=j: e.transpose(psb[b][:, j * 128:(j + 1) * 128], pgs[s3][:, j * 128:(j + 1) * 128], ident[:]), r=[f'pgs{s3}', 'ident'], w=[f'ps{b}'])
                            A('act', lambda e, b=b, rt=rt: e.copy(kTw[:, :, rt * 128:(rt + 1) * 128], psb[b][:, 0:512].rearrange("p (a t) -> p a t", a=4)), r=[f'ps{b}'], w=['kTw'])
                            A('pool', lambda e, s3=s3, rt=rt: e.tensor_copy(Vw[:, rt, :], pgs[s3][:, 512:1024]), r=[f'pgs{s3}'], w=['Vw'])
                        for bi in range(2):
                            A('pool', lambda e, bi=bi, n=n: e.dma_start(out=vnew[bi][:], in_=kv_o[1 + bi][SEQ + 4 * n:SEQ + 4 * n + 4, 512:1024]), r=[f'kvo{1 + bi}'], w=[f'vnew{bi}'], dma=True)
                        A('sp', lambda e, n=n: e.dma_start(out=win_o[n, 0:508, :], in_=state_win[n, 4:512, :]), w=['win_o'], dma=True)
                        A('sp', lambda e, n=n: e.dma_start(out=win_o[n, 508:512, :], in_=kv_o[2][SEQ + 4 * n:SEQ + 4 * n + 4, :]), r=['kvo2'], w=['win_o'], dma=True)
                        for j in range(4):
                            rq = qT[:, 4 * j:4 * j + 4, qc:qc + 4]
                            for bi in range(2):
                                ntile = 16 if bi == 0 else 4
                                kTx = kTs if bi == 0 else kTw
                                Vx_ = Vs if bi == 0 else Vw
                                kkey, vkey = ('kTs', 'Vs') if bi == 0 else ('kTw', 'Vw')
                                if bi == 0:
                                    bs_ = 2 + j % 2
                                    A('pe', lambda e, n=n, j=j, bs_=bs_: e.matmul(ps[bs_][:, 0:132], lhsT=selN[:, n, :], rhs=Dm[:, j].rearrange("p b t -> p (b t)"), start=True, stop=True), r=['selN', 'Dm'], w=[f'ps{bs_}'])
                                    sr = ps[bs_][:, 0:132].rearrange("p (b t) -> p b t", t=4)
                                    sr2 = ps[bs_][:, 0:128].rearrange("p (a c t) -> p a c t", c=2, t=4)
                                    A('dve', lambda e, sr2=sr2: e.tensor_scalar(mks[:], sr2[:, :, 0, :], hb0, None, op0=ALU.mult), r=[f'ps{bs_}', 'misc'], w=['mks'])
                                    A('dve', lambda e, sr2=sr2: e.scalar_tensor_tensor(out=mks[:], in0=sr2[:, :, 1, :], scalar=hb, in1=mks[:], op0=ALU.mult, op1=ALU.add), r=[f'ps{bs_}', 'misc', 'mks'], w=['mks'])
                                    mk_ap = mks[:]
                                    mkkey = 'mks'
                                else:
                                    mk_ap = mwb[:]
                                    mkkey = 'mwb'
                                bsc = 4 + (j * 2 + bi) % 2
                                for tl in range(ntile):
                                    A('pe', lambda e, bsc=bsc, tl=tl, j=j, rq=rq, kTx=kTx: e.matmul(ps[bsc][:, tl * 16:(tl + 1) * 16].rearrange("p (g t) -> p g t", g=4), lhsT=kTx[:, j, tl * 128:(tl + 1) * 128], rhs=rq, start=True, stop=True),
                                      r=[kkey, 'qT'], w=[f'ps{bsc}'])
                                A('pe', lambda e, bsc=bsc, j=j, rq=rq, bi=bi, n=n: e.matmul(ps[bsc][0:4, 256:272].rearrange("p (g t) -> p g t", g=4), lhsT=knT[bi][:, j, 4 * n:4 * n + 4], rhs=rq, start=True, stop=True),
                                  r=[f'knT{bi}', 'qT'], w=[f'ps{bsc}'])
                                A('act', lambda e, bsc=bsc, ntile=ntile: e.activation(out=pes[:, 0:ntile].rearrange("p a g t -> p (a g t)"), in_=ps[bsc][:, 0:ntile * 16], func=AF.Exp, scale=SCALE), r=[f'ps{bsc}'], w=['pes'])
                                A('act', lambda e, bsc=bsc: e.activation(out=pnw[:].rearrange("p g t -> p (g t)"), in_=ps[bsc][0:4, 256:272], func=AF.Exp, scale=SCALE), r=[f'ps{bsc}'], w=['pnw'])
                                for g in range(4):
                                    A('dve', lambda e, g=g, ntile=ntile, mk_ap=mk_ap: e.tensor_tensor(out=Pm[:, 0:ntile, g, :], in0=pes[:, 0:ntile, g, :], in1=mk_ap[:, 0:ntile, :], op=ALU.mult), r=['pes', mkkey], w=['Pm'])
                                    if bi == 0:
                                        A('dve', lambda e, g=g, sr=sr: e.tensor_tensor(out=pnw[:, g, :], in0=pnw[:, g, :], in1=sr[0:4, 32, :], op=ALU.mult), r=['pnw', f'ps{bs_}'], w=['pnw'])
                                    A('dve', lambda e, g=g: e.tensor_tensor(out=Pnf[:, g, :], in0=pnw[:, g, :], in1=cnew[:, g * 4:(g + 1) * 4], op=ALU.mult), r=['pnw', 'misc'], w=['Pnf'])
                                A('act', lambda e: e.copy(Pn[:], Pnf[:]), r=['Pnf'], w=['Pn'])
                                bo = 6 + (j * 2 + bi) % 2
                                for tl in range(ntile):
                                    A('pe', lambda e, bo=bo, tl=tl, j=j, Vx_=Vx_: e.matmul(ps[bo][:, tl * 16:(tl + 1) * 16], lhsT=Vx_[:, tl, j * 128:(j + 1) * 128], rhs=Pm[:, tl].rearrange("p g t -> p (g t)"), start=True, stop=True),
                                      r=[vkey, 'Pm'], w=[f'ps{bo}'])
                                A('pe', lambda e, bo=bo, j=j, bi=bi, ntile=ntile: e.matmul(ps[bo][:, ntile * 16:(ntile + 1) * 16], lhsT=vnew[bi][:, j * 128:(j + 1) * 128], rhs=Pn[:].rearrange("p g t -> p (g t)"), start=True, stop=True),
                                  r=[f'vnew{bi}', 'Pn'], w=[f'ps{bo}'])
                                A('dve', lambda e, ntile=ntile: e.tensor_reduce(out=Psum[:], in_=Pm[:, 0:ntile].rearrange("p a g t -> p (g t) a"), axis=AX.X, op=ALU.add), r=['Pm'], w=['Psum'])
                                A('pe', lambda e, bo=bo: e.matmul(ps[bo][:, 320:336], lhsT=ones_f[:], rhs=Psum[:], start=True, stop=True), r=['ones_f', 'Psum'], w=[f'ps{bo}'])
                                A('pe', lambda e, bo=bo: e.matmul(ps[bo][:, 336:352], lhsT=ones_f[0:4, :], rhs=Pnf[:].rearrange("p g t -> p (g t)"), start=True, stop=True), r=['ones_f', 'Pnf'], w=[f'ps{bo}'])
                                A('dve', lambda e, bo=bo, ntile=ntile: e.tensor_reduce(out=osum[:], in_=ps[bo][:, 0:(ntile + 1) * 16].rearrange("p (a c) -> p c a", c=16), axis=AX.X, op=ALU.add), r=[f'ps{bo}'], w=['osum'])
                                A('dve', lambda e, bo=bo: e.tensor_copy(zr2[:], ps[bo][:, 320:336]), r=[f'ps{bo}'], w=['zr2'])
                                A('dve', lambda e, bo=bo: e.tensor_tensor(out=zr2[:], in0=zr2[:], in1=ps[bo][:, 336:352], op=ALU.add), r=[f'ps{bo}', 'zr2'], w=['zr2'])
                                A('dve', lambda e: e.tensor_scalar(zr2[:], zr2[:], 1e-30, None, op0=ALU.max), r=['zr2'], w=['zr2'])
                                A('dve', lambda e: e.reciprocal(zr2[:], zr2[:]), r=['zr2'], w=['zr2'])
                                A('dve', lambda e, bi=bi, j=j, n=n: e.tensor_tensor(out=OTb[1 + bi][:, 4 * j:4 * j + 4, n, :], in0=osum[:].rearrange("p (g t) -> p g t", g=4), in1=zr2[:].rearrange("p (g t) -> p g t", g=4), op=ALU.mult),
                                  r=['osum', 'zr2'], w=[f'OTb{1 + bi}'])
                    ats = sb(stn, "ats", [64, 16, 128])
                    atsb = sb(stn, "atsb", [64, 16, 128], BF16)
                    sjunk = sb(stn, "sjunk", [64, 2048], BF16)
                    sssq = sb(stn, "sssq", [64, 16])
                    for br in range(3):
                        for hq in range(4):
                            b = (br * 4 + hq) % 2
                            for h4 in range(4):
                                hh = hq * 4 + h4
                                A('pe', lambda e, b=b, br=br, hh=hh, h4=h4: e.transpose(ps[b][0:64, h4 * 128:(h4 + 1) * 128], OTb[br][:, hh].rearrange("p n t -> p (n t)"), identf[:]), r=[f'OTb{br}', 'identf'], w=[f'ps{b}'])
                            for h4 in range(4):
                                hh = hq * 4 + h4
                                gcol = gates[0:64, 8, br * 16 + hh:br * 16 + hh + 1]
                                if br == 0:
                                    A('dve', lambda e, b=b, hh=hh, h4=h4, gcol=gcol: e.tensor_scalar(ats[:, hh, :], ps[b][0:64, h4 * 128:(h4 + 1) * 128], gcol, None, op0=ALU.mult), r=[f'ps{b}', 'gates'], w=['ats'])
                                else:
                                    A('dve', lambda e, b=b, hh=hh, h4=h4, gcol=gcol: e.scalar_tensor_tensor(out=ats[:, hh, :], in0=ps[b][0:64, h4 * 128:(h4 + 1) * 128], scalar=gcol, in1=ats[:, hh, :], op0=ALU.mult, op1=ALU.add),
                                      r=[f'ps{b}', 'gates', 'ats'], w=['ats'])
                    A('dve', lambda e: e.memset(sssq[:, 0:1], 0.0), w=['sssq'])
                    A('act', lambda e: e.activation(out=sjunk[:], in_=ats[:].rearrange("p h d -> p (h d)"), func=AF.Square, accum_out=sssq[:, 0:1]), r=['ats', 'sssq'], w=['sjunk', 'sssq'])
                    A('act', lambda e: e.activation(out=inv_a[0:64, 8:9], in_=sssq[:, 0:1], func=AF.Sqrt, bias=epsc[0:64, :], scale=1.0 / 2048), r=['sssq', 'epsc'], w=['inv_a'])
                    A('dve', lambda e: e.reciprocal(inv_a[0:64, 8:9], inv_a[0:64, 8:9]), r=['inv_a'], w=['inv_a'])
                    A('act', lambda e: e.copy(atsb[:], ats[:]), r=['ats'], w=['atsb'])
                    for g2 in range(2):
                        b = 2 + g2
                        for c in range(8):
                            A('pe', lambda e, b=b, g2=g2, c=c: e.transpose(psb[b][:, c * 64:(c + 1) * 64], atsb[:, g2 * 8 + c, :], ident[0:64, 0:64]), r=['atsb', 'ident'], w=[f'ps{b}'])
                        A('dve', lambda e, b=b, g2=g2: e.tensor_tensor(out=attnT[:, g2 * 8:(g2 + 1) * 8, SC0:SC0 + NST], in0=psb[b][:, 0:512].rearrange("p (c t) -> p c t", c=8),
                                                                     in1=bc(goutT[:, 16 + g2 * 8:16 + (g2 + 1) * 8].unsqueeze(2), [128, 8, NST]), op=ALU.mult), r=[f'ps{b}', 'cA'], w=['attnT'])
                    P.emit()

        OT = [(n, 128 * n, 128, 128 * n) for n in range(8)] + [(8, SEQ, NST, HALF)]
        if upto >= 6:
            with ExitStack() as st:
                cvT = sb(st, "cvT", [128, 16, NOUT], BF16)
                wo = [sb(st, f"wo{i}", [128, NKC, 512], BF16) for i in range(2)]
                xt_ = [sb(st, f"xt{i}", [128, 512]) for i in range(3)]
                ht = [sb(st, f"ht{i}", [128, 512]) for i in range(3)]
                A('sp', lambda e: e.dma_start(out=cvT[:], in_=convT_s.rearrange("j p n -> p j n")), w=['cvT'], dma=True)
                n_ = 0
                for cg in range(8):
                    s2 = cg % 2
                    A('pool', lambda e, s2=s2, cg=cg: e.dma_start(out=wo[s2][:], in_=w_out[:, cg * 512:(cg + 1) * 512].rearrange("(c p) n -> p c n", p=128)), w=[f'wo{s2}'], dma=True)
                    for (n, r0, R, o0) in OT:
                        bA, bB = (n_ % 4) * 2, (n_ % 4) * 2 + 1
                        s3 = n_ % 3
                        n_ += 1
                        for c in range(16):
                            A('pe', lambda e, bA=bA, s2=s2, c=c, R=R, o0=o0: e.matmul(ps[bA][0:R, :], lhsT=cvT[:, c, o0:o0 + R], rhs=wo[s2][:, c, :], start=(c == 0), stop=(c == 15)), r=['cvT', f'wo{s2}'], w=[f'ps{bA}'])
                        for c in range(16):
                            A('pe', lambda e, bB=bB, s2=s2, c=c, R=R, o0=o0: e.matmul(ps[bB][0:R, :], lhsT=attnT[:, c, o0:o0 + R], rhs=wo[s2][:, 16 + c, :], start=(c == 0), stop=(c == 15)), r=['attnT', f'wo{s2}'], w=[f'ps{bB}'])
                        A('sp', lambda e, s3=s3, r0=r0, R=R, cg=cg: e.dma_start(out=xt_[s3][0:R, :], in_=xs[r0:r0 + R, cg * 512:(cg + 1) * 512]), w=[f'xt{s3}'], dma=True)
                        A('dve', lambda e, bA=bA, s3=s3, R=R, n=n: e.scalar_tensor_tensor(out=ht[s3][0:R, :], in0=ps[bA][0:R, :], scalar=inv_c[0:R, n:n + 1], in1=xt_[s3][0:R, :], op0=ALU.mult, op1=ALU.add),
                          r=[f'ps{bA}', 'inv_c', f'xt{s3}'], w=[f'ht{s3}'])
                        A('dve', lambda e, bB=bB, s3=s3, R=R, n=n: e.scalar_tensor_tensor(out=ht[s3][0:R, :], in0=ps[bB][0:R, :], scalar=inv_a[0:R, n:n + 1], in1=ht[s3][0:R, :], op0=ALU.mult, op1=ALU.add),
                          r=[f'ps{bB}', 'inv_a', f'ht{s3}'], w=[f'ht{s3}'])
                        A('sp', lambda e, s3=s3, R=R, o0=o0, cg=cg: e.dma_start(out=h_s[o0:o0 + R, cg * 512:(cg + 1) * 512], in_=ht[s3][0:R, :]), r=[f'ht{s3}'], w=['h_s'], dma=True)
                P.emit()

        with ExitStack() as st:
          if DBG_DUMP:
            dt_ = sb(st, "dbgt", [128, 4096])
            A('dve', lambda e: e.memset(dt_[:], 0.0), w=['dbgt'])
            A('dve', lambda e: e.tensor_copy(dt_[:, 0:9 * 48], gates[:].rearrange("p a b -> p (a b)")), r=['gates'], w=['dbgt'])
            A('dve', lambda e: e.tensor_copy(dt_[:, 512:521], inv_c[:]), r=['inv_c'], w=['dbgt'])
            A('dve', lambda e: e.tensor_copy(dt_[:, 1024:1024 + NA], qT[:, 3, :]), r=['qT'], w=['dbgt'])
            A('dve', lambda e: e.tensor_copy(dt_[:, 2176:2176 + NOUT], attnT[:, DBG_HEAD, :]), r=['attnT'], w=['dbgt'])
            A('dve', lambda e: e.tensor_copy(dt_[:, 600:609], inv_a[:]), r=['inv_a'], w=['dbgt'])
            A('dve', lambda e: e.tensor_copy(dt_[:, 3300:3316], attnT[:, :, 40]), r=['attnT'], w=['dbgt'])
            A('dve', lambda e: e.tensor_copy(dt_[:, 3316:3332], attnT[:, :, 700]), r=['attnT'], w=['dbgt'])
            A('sp', lambda e: e.dma_start(out=dbg, in_=dt_[:]), r=['dbgt'], w=['dbg'], dma=True)
            P.emit()
        st_mid.close()

        if upto >= 7:
            PASSES = [OT[0:4], OT[4:9]]
            for pi, ptiles in enumerate(PASSES):
                ntok = sum(t[2] for t in ptiles)
                pc0 = ptiles[0][3]
                with ExitStack() as st:
                    xtT = sb(st, "xtT", [128, NKC, ntok], BF16)
                    hidT = sb(st, "hidT", [128, 64, ntok], BF16)
                    gateT = sb(st, "gateT", [16, ntok])
                    Sel = sb(st, "Sel", [16, 16, 128])
                    with ExitStack() as st2:
                        hin = [sb(st2, f"hin{i}", [128, D]) for i in range(2)]
                        hsc = sb(st2, "hsc", [128, D])
                        xtf = [sb(st2, f"xtf{i}", [128, 4, 128]) for i in range(3)]
                        wr = sb(st2, "wr", [128, NKC, 20])
                        rs = sb(st2, "rs", [128, 128])
                        el3 = sb(st2, "el3", [128, 4, 4])
                        g16 = sb(st2, "g16", [128, 4, 4])
                        lgT = sb(st2, "lgT", [20, 128])
                        A('sp', lambda e: e.dma_start(out=wr[:, :, 0:4], in_=w_gr.rearrange("(c p) n -> p c n", p=128)), w=['wr'], dma=True)
                        A('sp', lambda e: e.dma_start(out=wr[:, :, 4:20], in_=w_er.rearrange("(c p) n -> p c n", p=128)), w=['wr'], dma=True)
                        A('dve', lambda e: e.tensor_copy(Sel[:], bc(identf[0:16, 0:16].unsqueeze(2), [16, 16, 128])), r=['identf'], w=['Sel'])
                        for tn_, (n, r0, R, o0) in enumerate(ptiles):
                            s2 = tn_ % 2
                            tc0 = o0 - pc0
                            A('sp', lambda e, s2=s2, R=R, o0=o0: e.dma_start(out=hin[s2][0:R, :], in_=h_s[o0:o0 + R, :]), r=['h_s'], w=[f'hin{s2}'], dma=True)
                            A('dve', lambda e: e.memset(rs[:, 0:1], 0.0), w=['rs0'])
                            A('act', lambda e, s2=s2, R=R: e.activation(out=hsc[0:R, :], in_=hin[s2][0:R, :], func=AF.Square, accum_out=rs[0:R, 0:1]), r=[f'hin{s2}'], w=['hsc', 'rs0'])
                            A('act', lambda e, R=R: e.activation(out=rs[0:R, 0:1], in_=rs[0:R, 0:1], func=AF.Sqrt, bias=epsc[0:R, :], scale=1.0 / D), r=['rs0', 'epsc'], w=['rs0'])
                            A('dve', lambda e, R=R: e.reciprocal(rs[0:R, 0:1], rs[0:R, 0:1]), r=['rs0'], w=['rs0'])
                            A('dve', lambda e, s2=s2, R=R: e.tensor_scalar(hsc[0:R, :], hin[s2][0:R, :], rs[0:R, 0:1], None, op0=ALU.mult), r=[f'hin{s2}', 'rs0'], w=['hsc'])
                            for g in range(8):
                                b = g % 2
                                s3 = g % 3
                                for c in range(4):
                                    kc = g * 4 + c
                                    A('pe', lambda e, b=b, c=c, kc=kc, R=R: e.transpose(ps[b][:, c * 128:c * 128 + R], hsc[0:R, kc * 128:(kc + 1) * 128], identf[0:R, 0:R]), r=['hsc', 'identf'], w=[f'ps{b}'])
                                A('dve', lambda e, b=b, s3=s3, g=g, R=R: e.tensor_tensor(out=xtf[s3][:, :, 0:R], in0=ps[b][:].rearrange("p (c t) -> p c t", c=4)[:, :, 0:R],
                                                                                    in1=bc(gffnT[:, g * 4:(g + 1) * 4].unsqueeze(2), [128, 4, R]), op=ALU.mult), r=[f'ps{b}', 'cA'], w=[f'xtf{s3}'])
                                A('act', lambda e, s3=s3, g=g, R=R, tc0=tc0: e.copy(xtT[:, g * 4:(g + 1) * 4, tc0:tc0 + R], xtf[s3][:, :, 0:R]), r=[f'xtf{s3}'], w=['xtT'])
                                for c in range(4):
                                    kc = g * 4 + c
                                    A('pe', lambda e, s3=s3, c=c, kc=kc, R=R: e.matmul(ps[2][0:20, 0:R], lhsT=wr[:, kc, :], rhs=xtf[s3][:, c, 0:R], start=(kc == 0), stop=(kc == NKC - 1)), r=[f'xtf{s3}', 'wr'], w=['ps2'])
                            lg = rs[:, 8:28]
                            A('act', lambda e, R=R: e.copy(lgT[:, 0:R], ps[2][0:20, 0:R]), r=['ps2'], w=['lgT'])
                            A('pe', lambda e, R=R: e.transpose(ps[3][0:R, 0:20], lgT[:, 0:R], identf[0:20, 0:20]), r=['lgT', 'identf'], w=['ps3'])
                            A('dve', lambda e, R=R: e.tensor_tensor(out=rs[0:R, 8:28], in0=ps[3][0:R, 0:20], in1=br_bc[0:R, :], op=ALU.add), r=['ps3', 'br_bc'], w=['rs_lg'])
                            A('dve', lambda e, R=R: e.tensor_reduce(out=rs[0:R, 1:2], in_=rs[0:R, 8:12], axis=AX.X, op=ALU.max), r=['rs_lg'], w=['rs_m4'])
                            A('dve', lambda e, R=R: e.tensor_scalar(rs[0:R, 28:32], rs[0:R, 8:12], rs[0:R, 1:2], None, op0=ALU.is_equal), r=['rs_lg', 'rs_m4'], w=['rs_oh'])
                            A('dve', lambda e, R=R: e.tensor_scalar(rs[0:R, 2:3], rs[0:R, 1:2], -1.0, None, op0=ALU.mult), r=['rs_m4'], w=['rs_nm4'])
                            A('dve', lambda e, R=R: e.memset(rs[0:R, 3:4], 0.0), w=['rs_sg'])
                            A('act', lambda e, R=R: e.activation(out=rs[0:R, 32:36], in_=rs[0:R, 8:12], func=AF.Exp, bias=rs[0:R, 2:3], scale=1.0, accum_out=rs[0:R, 3:4]), r=['rs_lg', 'rs_nm4', 'rs_sg'], w=['rs_eg', 'rs_sg'])
                            A('dve', lambda e, R=R: e.reciprocal(rs[0:R, 3:4], rs[0:R, 3:4]), r=['rs_sg'], w=['rs_sg'])
                            A('dve', lambda e, R=R: e.tensor_tensor(out=el3[0:R], in0=rs[0:R, 12:28].rearrange("p (g x) -> p g x", g=4), in1=bc(rs[0:R, 28:32].unsqueeze(2), [R, 4, 4]), op=ALU.mult), r=['rs_lg', 'rs_oh'], w=['el3'])
                            A('dve', lambda e, R=R: e.tensor_reduce(out=rs[0:R, 36:40], in_=el3[0:R].rearrange("p g x -> p x g"), axis=AX.X, op=ALU.add), r=['el3'], w=['rs_es'])
                            A('dve', lambda e, R=R: e.tensor_reduce(out=rs[0:R, 4:5], in_=rs[0:R, 36:40], axis=AX.X, op=ALU.max), r=['rs_es'], w=['rs_me'])
                            A('dve', lambda e, R=R: e.tensor_scalar(rs[0:R, 4:5], rs[0:R, 4:5], -1.0, None, op0=ALU.mult), r=['rs_me'], w=['rs_me'])
                            A('dve', lambda e, R=R: e.memset(rs[0:R, 40:48], 0.0), w=['rs_ee'])
                            A('act', lambda e, R=R: e.activation(out=rs[0:R, 40:44], in_=rs[0:R, 36:40], func=AF.Exp, bias=rs[0:R, 4:5], scale=1.0), r=['rs_es', 'rs_me', 'rs_ee'], w=['rs_ee'])
                            A('dve', lambda e, R=R: e.max(out=rs[0:R, 48:56], in_=rs[0:R, 40:48]), r=['rs_ee'], w=['rs_mx'])
                            A('dve', lambda e, R=R: e.tensor_scalar(rs[0:R, 56:60], rs[0:R, 40:44], rs[0:R, 49:50], None, op0=ALU.is_ge), r=['rs_ee', 'rs_mx'], w=['rs_m2'])
                            A('dve', lambda e, R=R: e.tensor_tensor(out=rs[0:R, 56:60], in0=rs[0:R, 56:60], in1=rs[0:R, 40:44], op=ALU.mult), r=['rs_m2', 'rs_ee'], w=['rs_m2'])
                            A('dve', lambda e, R=R: e.tensor_reduce(out=rs[0:R, 5:6], in_=rs[0:R, 56:60], axis=AX.X, op=ALU.add), r=['rs_m2'], w=['rs_s2'])
                            A('dve', lambda e, R=R: e.reciprocal(rs[0:R, 5:6], rs[0:R, 5:6]), r=['rs_s2'], w=['rs_s2'])
                            A('dve', lambda e, R=R: e.tensor_tensor(out=rs[0:R, 5:6], in0=rs[0:R, 5:6], in1=rs[0:R, 3:4], op=ALU.mult), r=['rs_s2', 'rs_sg'], w=['rs_s2'])
                            A('dve', lambda e, R=R: e.tensor_scalar(rs[0:R, 56:60], rs[0:R, 56:60], rs[0:R, 5:6], None, op0=ALU.mult), r=['rs_m2', 'rs_s2'], w=['rs_m2'])
                            for gq in range(4):
                                A('dve', lambda e, R=R, gq=gq: e.tensor_scalar(g16[0:R, gq, :], rs[0:R, 56:60], rs[0:R, 28 + gq:29 + gq], None, op0=ALU.mult), r=['rs_m2', 'rs_oh'], w=['g16'])
                            A('pe', lambda e, R=R: e.transpose(ps[3][0:16, 0:R], g16[0:R].rearrange("p g x -> p (g x)"), identf[0:R, 0:R]), r=['g16', 'identf'], w=['ps3'])
                            A('act', lambda e, R=R, tc0=tc0: e.copy(gateT[:, tc0:tc0 + R], ps[3][0:16, 0:R]), r=['ps3'], w=['gateT'])
                        P.emit()
                    with ExitStack() as st2:
                        wgu = [sb(st2, f"wgu{i}", [128, 2, NKC, 128], BF16) for i in range(3)]
                        sgt = [sb(st2, f"sgt{i}", [128, 512]) for i in range(2)]
                        TGS = [(0, 512)] + ([(512, ntok - 512)] if ntok > 512 else [])
                        n_ = 0
                        for ex in range(16):
                            for fc in range(4):
                                s3 = (ex * 4 + fc) % 3
                                A('pool', lambda e, s3=s3, ex=ex, fc=fc: e.dma_start(out=wgu[s3][:, 0], in_=w_gate[ex][:, fc * 128:(fc + 1) * 128].rearrange("(c p) n -> p c n", p=128)), w=[f'wgu{s3}'], dma=True)
                                A('pool', lambda e, s3=s3, ex=ex, fc=fc: e.dma_start(out=wgu[s3][:, 1], in_=w_up[ex][:, fc * 128:(fc + 1) * 128].rearrange("(c p) n -> p c n", p=128)), w=[f'wgu{s3}'], dma=True)
                                for (t0, tn) in TGS:
                                    b0 = (n_ % 2) * 3
                                    k2 = n_ % 2
                                    n_ += 1
                                    for gu in range(2):
                                        for c in range(NKC):
                                            A('pe', lambda e, b=b0 + gu, s3=s3, gu=gu, c=c, t0=t0, tn=tn: e.matmul(ps[b][:, 0:tn], lhsT=wgu[s3][:, gu, c, :], rhs=xtT[:, c, t0:t0 + tn], start=(c == 0), stop=(c == NKC - 1)),
                                              r=[f'wgu{s3}', 'xtT'], w=[f'ps{b0 + gu}'])
                                    A('pe', lambda e, b0=b0, ex=ex, t0=t0, tn=tn: e.matmul(ps[b0 + 2][:, 0:tn], lhsT=Sel[:, ex, :], rhs=gateT[:, t0:t0 + tn], start=True, stop=True), r=['Sel', 'gateT'], w=[f'ps{b0 + 2}'])
                                    A('act', lambda e, b0=b0, k2=k2, tn=tn: e.activation(out=sgt[k2][:, 0:tn], in_=ps[b0][:, 0:tn], func=AF.Silu), r=[f'ps{b0}'], w=[f'sgt{k2}'])
                                    A('dve', lambda e, b0=b0, k2=k2, tn=tn: e.tensor_tensor(out=sgt[k2][:, 0:tn], in0=sgt[k2][:, 0:tn], in1=ps[b0 + 1][:, 0:tn], op=ALU.mult), r=[f'sgt{k2}', f'ps{b0 + 1}'], w=[f'sgt{k2}'])
                                    A('dve', lambda e, b0=b0, k2=k2, ex=ex, fc=fc, t0=t0, tn=tn: e.tensor_tensor(out=hidT[:, ex * 4 + fc, t0:t0 + tn], in0=sgt[k2][:, 0:tn], in1=ps[b0 + 2][:, 0:tn], op=ALU.mult),
                                      r=[f'sgt{k2}', f'ps{b0 + 2}'], w=['hidT'])
                        P.emit()
                    with ExitStack() as st2:
                        wd = [sb(st2, f"wd{i}", [128, 64, 256], BF16) for i in range(2)]
                        hres = [sb(st2, f"hres{i}", [128, 256]) for i in range(3)]
                        n_ = 0
                        for dg in range(16):
                            s2 = dg % 2
                            A('pool', lambda e, s2=s2, dg=dg: e.dma_start(out=wd[s2][:], in_=w_down[:, :, dg * 256:(dg + 1) * 256].rearrange("e (f p) n -> p (e f) n", p=128)), w=[f'wd{s2}'], dma=True)
                            for (n, r0, R, o0) in ptiles:
                                b = n_ % 4
                                s3 = n_ % 3
                                n_ += 1
                                tc0 = o0 - pc0
                                for k in range(64):
                                    A('pe', lambda e, b=b, s2=s2, k=k, R=R, tc0=tc0: e.matmul(ps[b][0:R, 0:256], lhsT=hidT[:, k, tc0:tc0 + R], rhs=wd[s2][:, k, :], start=(k == 0), stop=(k == 63)), r=['hidT', f'wd{s2}'], w=[f'ps{b}'])
                                A('sp', lambda e, s3=s3, R=R, o0=o0, dg=dg: e.dma_start(out=hres[s3][0:R, :], in_=h_s[o0:o0 + R, dg * 256:(dg + 1) * 256]), r=['h_s'], w=[f'hres{s3}'], dma=True)
                                A('dve', lambda e, b=b, s3=s3, R=R: e.tensor_tensor(out=hres[s3][0:R, :], in0=ps[b][0:R, 0:256], in1=hres[s3][0:R, :], op=ALU.add), r=[f'ps{b}', f'hres{s3}'], w=[f'hres{s3}'])
                                A('sp', lambda e, s3=s3, R=R, o0=o0, dg=dg: e.dma_start(out=y_o[o0:o0 + R, dg * 256:(dg + 1) * 256], in_=hres[s3][0:R, :]), r=[f'hres{s3}'], w=['y_o'], dma=True)
                        P.emit()

    return nc


def host_consts(c):
    import ml_dtypes
    bf = ml_dtypes.bfloat16
    h = c % 2
    p0 = HALF * h
    pos = p0 + np.arange(HALF)
    cp = np.arange(128)
    nat = (cp + 64 * h) % 128
    m_cmpT = ((nat[:, None] <= 126) & ((16 * nat[:, None] + 31) <= pos[None, :])).astype(np.float32)
    i_ = np.arange(128)[:, None]
    j_ = np.arange(33)[None, :]
    Mnat = np.zeros((128, 33), np.float32)
    for a in range(4):
        for cc in range(2):
            Mnat += (i_ == 4 * j_ + a - cc)
    Mnat[127, :] = 0
    jn = (np.arange(32) + 16 * h) % 32
    MZ = np.concatenate([Mnat[nat][:, jn] * (nat[:, None] <= 126), np.ones((128, 1), np.float32)], axis=1)
    cur = pos // 64
    jb = jn[None, :]
    forced = (jb == 0) | (jb == cur[:, None]) | (jb == cur[:, None] - 1)
    causal = jb <= cur[:, None]
    addm = np.where(causal, np.where(forced, 1000.0, 0.0), NEG).astype(np.float32)
    m_add = addm.reshape(8, 128, 32).transpose(1, 0, 2)
    m_cblk = causal.astype(np.float32).reshape(8, 128, 32).transpose(1, 0, 2)
    m_E = (np.arange(SEQ)[None, :] // 64 == np.arange(32)[:, None]).astype(np.float32)
    k_ = np.arange(128)[:, None]
    q_ = np.arange(128)[None, :]
    m_tri = np.concatenate([(k_ <= q_), (k_ > q_)], axis=1).astype(np.float32)
    m_flag = np.full((128, 1), float(h), np.float32)
    s_MZ = np.concatenate([Mnat, np.ones((128, 1), np.float32)], axis=1)
    s_MZ[127, :] = 0
    s_add = np.zeros((64, 40), np.float32)
    s_add[:, 33:] = NEG
    s_add[:, [0, 31, 32]] = 1000.0
    s_misc = np.zeros((128, 64), np.float32)
    s_misc[:, 0] = (np.arange(128) >= 64)
    s_misc[:, 1] = 1 - s_misc[:, 0]
    s_misc[:, 2] = (np.arange(128) <= 126)
    t_of = np.arange(64) % 4
    s_misc[:64, 4:8] = (t_of[:, None] == np.arange(4)[None, :])
    s_misc[:4, 8:24] = np.tile((np.arange(4)[:, None] <= np.arange(4)[None, :]), (1, 4))
    s_misc[:, 24:40] = ((128 * np.arange(4)[None, :, None] + np.arange(128)[:, None, None]) > np.arange(4)[None, None, :]).reshape(128, 16)
    s_selN = (np.arange(64)[:, None, None] // 4 == np.arange(NS)[None, :, None]).astype(np.float32) * np.ones((1, 1, 128), np.float32)
    return dict(m_cmpT=m_cmpT.astype(bf), m_MZ=MZ.astype(bf), m_add=np.ascontiguousarray(m_add), m_cblk=np.ascontiguousarray(m_cblk),
                m_E=m_E.astype(bf), m_tri=m_tri.astype(bf), m_flag=m_flag, s_MZ=s_MZ.astype(bf), s_add=s_add, s_misc=s_misc,
                s_selN=s_selN.reshape(64, NS * 128).astype(bf))


def make_in_maps(inp, cores=None):
    f = lambda a: np.ascontiguousarray(np.asarray(a))
    has = lambda k: k in inp
    shared = {}

    def put(name, key, fn):
        if has(key):
            shared[name] = f(fn(inp[key]))
    put('w_in', 'w_in', lambda a: a[0])
    put('norm_mix_g', 'norm_mix_g', lambda a: a[0].reshape(32, 128))
    put('out_norm_g', 'out_norm_g', lambda a: a[0].reshape(32, 128))
    put('norm_ffn_g', 'norm_ffn_g', lambda a: a[0].reshape(32, 128))
    put('k_norm_g', 'k_norm_g', lambda a: a[0])
    put('q_norm_g', 'q_norm_g', lambda a: a[0].reshape(1, 128))
    put('conv_w', 'conv_w', lambda a: a[0].reshape(48, 128))
    put('phi_pe', 'phi_pe', lambda a: a[0].reshape(64, 128))
    put('phi_w1', 'phi_w1', lambda a: a[0])
    put('phi_w2', 'phi_w2', lambda a: a[0])
    put('cache_cmp', 'cache_cmp_kv', lambda a: a[0].reshape(2560 * 128, 1024))
    put('cache_sel', 'cache_sel_kv', lambda a: a[0].reshape(2560 * 128, 1024))
    put('w_out', 'w_out', lambda a: a[0])
    put('w_gr', 'w_group_router', lambda a: a[0])
    put('w_er', 'w_expert_router', lambda a: a[0])
    put('w_gate', 'w_gate', lambda a: a[0])
    put('w_up', 'w_up', lambda a: a[0])
    put('w_down', 'w_down', lambda a: a[0])
    if has('b_group_router'):
        shared['b_r'] = f(np.concatenate([np.asarray(inp['b_group_router'])[0], np.asarray(inp['b_expert_router'])[0]]).reshape(1, 20))
    maps = []
    xp = np.asarray(inp['x_prompt'])
    xsm = np.asarray(inp['x_sample'])
    for c in (range(NCORES) if cores is None else cores):
        b, h = c // 2, c % 2
        own = xp[b, h * HALF:(h + 1) * HALF]
        oth = xp[b, (1 - h) * HALF:(2 - h) * HALF]
        prev = xp[b, HALF - 2:HALF] if h == 1 else np.zeros((2, D), np.float32)
        xs = np.concatenate([own, oth, xsm[NS * c:NS * (c + 1)].reshape(NST, D), prev], axis=0)
        m = dict(shared)
        m['xs'] = f(xs)
        if has('state_conv'):
            m['state_conv'] = f(np.asarray(inp['state_conv'])[0, NS * c:NS * (c + 1)].reshape(512, 128))
        if has('state_win_kv'):
            m['state_win'] = f(np.asarray(inp['state_win_kv'])[0, NS * c:NS * (c + 1)].reshape(NS, 512, 1024))
        if has('page_table'):
            m['page_table'] = f(np.asarray(inp['page_table'])[NS * c:NS * (c + 1)].reshape(1, 256).astype(np.int32))
        m.update(host_consts(c))
        maps.append(m)
    return maps


def assemble(results):
    f32 = np.float32
    y_p = np.zeros((4, SEQ, D), f32)
    y_s = np.zeros((128, 4, D), f32)
    p_cmp = np.zeros((1, 4, SEQ, 2, 4, HD), f32)
    p_sel = np.zeros((1, 4, SEQ, 2, 4, HD), f32)
    p_win = np.zeros((1, 4, 512, 2, 4, HD), f32)
    p_conv = np.zeros((1, 4, 2, 2048), f32)
    s_cmp = np.zeros((1, 128, 4, 2, 4, HD), f32)
    s_sel = np.zeros((1, 128, 4, 2, 4, HD), f32)
    s_win = np.zeros((1, 128, 512, 2, 4, HD), f32)
    s_conv = np.zeros((1, 128, 2, 2048), f32)
    for c, r in enumerate(results):
        b, h = c // 2, c % 2
        sl = slice(h * HALF, (h + 1) * HALF)
        ss = slice(NS * c, NS * (c + 1))
        y_p[b, sl] = r['y'][0:HALF]
        y_s[ss] = r['y'][HALF:NOUT].reshape(NS, 4, D)
        p_cmp[0, b, sl] = r['kv0'][0:HALF].reshape(HALF, 2, 4, HD)
        p_sel[0, b, sl] = r['kv1'][0:HALF].reshape(HALF, 2, 4, HD)
        s_cmp[0, ss] = r['kv0'][SEQ:SEQ + NST].reshape(NS, 4, 2, 4, HD)
        s_sel[0, ss] = r['kv1'][SEQ:SEQ + NST].reshape(NS, 4, 2, 4, HD)
        s_win[0, ss] = r['win_o'].reshape(NS, 512, 2, 4, HD)
        cf = r['conv_o'].transpose(2, 0, 1).reshape(34, 2048)
        s_conv[0, ss] = cf[2:34].reshape(NS, 2, 2048)
        if h == 1:
            p_win[0, b] = r['kv2'][512:HALF].reshape(512, 2, 4, HD)
            p_conv[0, b] = cf[0:2]
    return (y_p, y_s, p_cmp, p_sel, p_win, p_conv, s_cmp, s_sel, s_win, s_conv)


def kernel(**inputs):
    nc = build()
    maps = make_in_maps(inputs)
    names = set()
    for alloc in nc.allocations:
        if isinstance(alloc, mybir.MemoryLocationSet) and alloc.kind == "ExternalInput":
            names.add(alloc.memorylocations[0].name)
    maps = [{k: v for k, v in m.items() if k in names} for m in maps]
    res = run_bass_kernel_spmd(nc, maps, core_ids=list(range(NCORES)))
    return assemble(res.results)


def make_in_maps_partial(inp, cores, nc):
    names = set()
    for alloc in nc.allocations:
        if isinstance(alloc, mybir.MemoryLocationSet) and alloc.kind == "ExternalInput":
            names.add(alloc.memorylocations[0].name)
    maps = make_in_maps(inp, cores)
    return [{k: v for k, v in m.items() if k in names} for m in maps]
```
